# Optimizing a Trainium2 kernel written in Bass

```python
import jax, jax.numpy as jnp
from jax import lax
import numpy as np

D_MODEL = 1024
BATCH = 4
SEQ = 4096
DEPTH = 2

N_MEM = 256
EPS = 1e-6
ROPE_THETA = 500000.0

SSD_HEADS = 8
SSD_HEAD_DIM = 64
SSD_INNER = SSD_HEADS * SSD_HEAD_DIM
SSD_GROUPS = 2
SSD_STATE = 128
SSD_CONV = 4
SSD_CHUNK = 128
SSD_CONV_DIM = SSD_INNER + 2 * SSD_GROUPS * SSD_STATE

HEAD_DIM = 64
ATTN_HEADS = 4
ATTN_INNER = ATTN_HEADS * HEAD_DIM
IDX_HEADS = 4
IDX_DIM = 64
TOPK_MAX = 256
Q_BLOCK = 128

MEM_HEADS = 4
MEM_INNER = MEM_HEADS * HEAD_DIM

D_MIX = SSD_INNER + ATTN_INNER + MEM_INNER

D_FF = 2816
FFN_CONV = 3

IN_SIZES = (SSD_INNER, SSD_CONV_DIM, SSD_HEADS,
            ATTN_INNER, ATTN_INNER, ATTN_INNER, IDX_HEADS * IDX_DIM, IDX_DIM, IDX_HEADS,
            MEM_INNER)
N_IN = sum(IN_SIZES)
IN_SPLITS = tuple(sum(IN_SIZES[:i + 1]) for i in range(len(IN_SIZES) - 1))

kernel_name = "hybrid_ssd_dsa_mem_convffn"


def rmsnorm(x, g):
    xf = x.astype(jnp.float32)
    r = lax.rsqrt(jnp.mean(xf * xf, axis=-1, keepdims=True) + EPS)
    return (xf * r).astype(x.dtype) * g


def rope_partial(x, pos):
    rot = x.shape[-1] // 4
    half = rot // 2
    inv_freq = ROPE_THETA ** (-(jnp.arange(half, dtype=jnp.float32) * 2.0 / rot))
    ang = pos.astype(jnp.float32)[..., None] * inv_freq
    cos = jnp.cos(ang)[:, :, None, :]
    sin = jnp.sin(ang)[:, :, None, :]
    xr = x[..., :rot].astype(jnp.float32)
    x1, x2 = xr[..., :half], xr[..., half:]
    rotated = jnp.concatenate([x1 * cos - x2 * sin, x2 * cos + x1 * sin], axis=-1)
    return jnp.concatenate([rotated.astype(x.dtype), x[..., rot:]], axis=-1)


def causal_dwconv(x, w, b):
    width = w.shape[0]
    y = lax.conv_general_dilated(
        x, w[:, None, :].astype(x.dtype), window_strides=(1,), padding=[(width - 1, 0)],
        dimension_numbers=('NWC', 'WIO', 'NWC'), feature_group_count=x.shape[-1])
    return y + b


def ssd_mixer(z, xbc, dt, conv_w, conv_b, dt_bias, a_log, d_skip, norm_g):
    bsz, L, _ = xbc.shape
    G, R, P, N, Q = SSD_GROUPS, SSD_HEADS // SSD_GROUPS, SSD_HEAD_DIM, SSD_STATE, SSD_CHUNK
    nc = L // Q
    xbc = jax.nn.silu(causal_dwconv(xbc, conv_w, conv_b))
    xs, bm, cm = jnp.split(xbc, [SSD_INNER, SSD_INNER + G * N], axis=-1)
    xs = xs.reshape(bsz, L, G, R, P)
    bm = bm.reshape(bsz, L, G, N)
    cm = cm.reshape(bsz, L, G, N)
    dt = jax.nn.softplus((dt + dt_bias).astype(jnp.float32)).reshape(bsz, L, G, R)
    a = -jnp.exp(a_log.astype(jnp.float32)).reshape(G, R)
    a_dt = dt * a
    xdt = xs.astype(jnp.float32) * dt[..., None]

    xc = xdt.reshape(bsz, nc, Q, G, R, P)
    bc = bm.reshape(bsz, nc, Q, G, N).astype(jnp.float32)
    cc = cm.reshape(bsz, nc, Q, G, N).astype(jnp.float32)
    ac = jnp.moveaxis(a_dt.reshape(bsz, nc, Q, G, R), 2, -1)
    cs = jnp.cumsum(ac, axis=-1)

    causal = jnp.tril(jnp.ones((Q, Q), dtype=bool))
    seg = cs[..., :, None] - cs[..., None, :]
    lmat = jnp.exp(jnp.where(causal, seg, -jnp.inf))
    cb = jnp.einsum('bclgn,bcsgn->bcgls', cc, bc)
    y_diag = jnp.einsum('bcgls,bcgrls,bcsgrp->bclgrp', cb, lmat, xc)

    decay = jnp.exp(cs[..., -1:] - cs)
    states = jnp.einsum('bclgn,bcgrl,bclgrp->bcgrpn', bc, decay, xc)
    chunk_decay = jnp.exp(cs[..., -1])

    def step(h, inp):
        st, dec = inp
        return h * dec[..., None, None] + st, h

    h0 = jnp.zeros((bsz, G, R, P, N), jnp.float32)
    _, prev = lax.scan(step, h0, (jnp.moveaxis(states, 1, 0), jnp.moveaxis(chunk_decay, 1, 0)))
    prev = jnp.moveaxis(prev, 0, 1)
    y_off = jnp.einsum('bclgn,bcgrpn,bcgrl->bclgrp', cc, prev, jnp.exp(cs))

    y = (y_diag + y_off).reshape(bsz, L, G, R, P)
    y = y + xs.astype(jnp.float32) * d_skip.astype(jnp.float32).reshape(G, R)[..., None]
    y = y.reshape(bsz, L, SSD_INNER).astype(z.dtype)
    yg = (y * jax.nn.silu(z)).reshape(bsz, L, G, SSD_INNER // G)
    return rmsnorm(yg, norm_g.reshape(G, SSD_INNER // G)).reshape(bsz, L, SSD_INNER)


def dsa_mixer(q, k, v, iq, ik, iw, pos, qn_g, kn_g):
    bsz, L, _ = q.shape
    q = rope_partial(rmsnorm(q.reshape(bsz, L, ATTN_HEADS, HEAD_DIM), qn_g), pos)
    k = rope_partial(rmsnorm(k.reshape(bsz, L, ATTN_HEADS, HEAD_DIM), kn_g), pos)
    v = v.reshape(bsz, L, ATTN_HEADS, HEAD_DIM)
    iq = rope_partial(iq.reshape(bsz, L, IDX_HEADS, IDX_DIM), pos)
    ik = rope_partial(ik[:, :, None, :], pos)[:, :, 0]
    iw = iw * ((IDX_HEADS ** -0.5) * (IDX_DIM ** -0.5))
    topk = min(TOPK_MAX, L // 4)
    n_blocks = L // Q_BLOCK
    key_pos = jnp.arange(L)
    scale = HEAD_DIM ** -0.5

    def block(i):
        q0 = i * Q_BLOCK
        qi = lax.dynamic_slice_in_dim(q, q0, Q_BLOCK, axis=1)
        iqi = lax.dynamic_slice_in_dim(iq, q0, Q_BLOCK, axis=1)
        iwi = lax.dynamic_slice_in_dim(iw, q0, Q_BLOCK, axis=1)
        qpos = q0 + jnp.arange(Q_BLOCK)
        rel = jax.nn.relu(jnp.einsum('bqhd,bsd->bqhs', iqi, ik).astype(jnp.float32))
        iscore = jnp.einsum('bqh,bqhs->bqs', iwi.astype(jnp.float32), rel)
        iscore = jnp.where(key_pos[None, None, :] <= qpos[None, :, None], iscore, -jnp.inf)
        _, sel = lax.top_k(iscore, topk)
        ksel = jax.vmap(lambda kk, ii: kk[ii])(k, sel)
        vsel = jax.vmap(lambda vv, ii: vv[ii])(v, sel)
        logits = jnp.einsum('bqhd,bqkhd->bhqk', qi, ksel).astype(jnp.float32) * scale
        valid = (sel <= qpos[None, :, None])[:, None]
        p = jax.nn.softmax(jnp.where(valid, logits, -jnp.inf), axis=-1)
        return jnp.einsum('bhqk,bqkhd->bqhd', p.astype(v.dtype), vsel)

    out = lax.map(block, jnp.arange(n_blocks))
    return jnp.moveaxis(out, 0, 1).reshape(bsz, L, ATTN_INNER)


def mem_mixer(mq, mem_kv, qn_g, kn_g):
    bsz, L, _ = mq.shape
    q = rmsnorm(mq.reshape(bsz, L, MEM_HEADS, HEAD_DIM), qn_g)
    mk, mv = jnp.split(mem_kv, 2, axis=-1)
    mk = rmsnorm(mk.reshape(bsz, -1, MEM_HEADS, HEAD_DIM), kn_g)
    mv = mv.reshape(bsz, -1, MEM_HEADS, HEAD_DIM)
    logits = jnp.einsum('bqhd,bmhd->bhqm', q, mk).astype(jnp.float32) * (HEAD_DIM ** -0.5)
    p = jax.nn.softmax(logits, axis=-1)
    return jnp.einsum('bhqm,bmhd->bqhd', p.astype(mv.dtype), mv).reshape(bsz, L, MEM_INNER)


def conv_ffn(h, w_up, conv_w, conv_b, w_down):
    u = causal_dwconv(h @ w_up, conv_w, conv_b)
    g, val = jnp.split(u, 2, axis=-1)
    return (jax.nn.silu(g) * val) @ w_down


def setup_inputs(seed: int = 0) -> dict:
    key = jax.random.key(seed)
    ks = jax.random.split(key, 32)
    f32 = jnp.float32

    def w(k, shape, fan_in):
        return jax.random.normal(k, shape, f32) * (fan_in ** -0.5)

    def gain(k, shape):
        return 1.0 + 0.02 * jax.random.normal(k, shape, f32)

    x = jax.random.normal(ks[0], (BATCH, SEQ, D_MODEL), f32)
    mem = jax.random.normal(ks[1], (BATCH, N_MEM, D_MODEL), f32)
    offset = jax.random.randint(ks[2], (BATCH, 1), 0, 1024, dtype=jnp.int32)
    pos = (offset + jnp.arange(SEQ, dtype=jnp.int32)[None, :]).astype(jnp.int32)

    dt0 = jnp.exp(jax.random.uniform(ks[6], (DEPTH, SSD_HEADS), f32)
                  * (jnp.log(0.1) - jnp.log(0.001)) + jnp.log(0.001))
    ssd_dt_bias = dt0 + jnp.log(-jnp.expm1(-dt0))
    ssd_a_log = jnp.log(jax.random.uniform(ks[7], (DEPTH, SSD_HEADS), f32, 1.0, 16.0))

    return {
        "x": x,
        "mem": mem,
        "pos": pos,
        "mix_norm_g": gain(ks[3], (DEPTH, D_MODEL)),
        "w_in": w(ks[4], (DEPTH, D_MODEL, N_IN), D_MODEL),
        "ssd_conv_w": 0.5 * jax.random.normal(ks[5], (DEPTH, SSD_CONV, SSD_CONV_DIM), f32),
        "ssd_conv_b": 0.01 * jax.random.normal(ks[8], (DEPTH, SSD_CONV_DIM), f32),
        "ssd_dt_bias": ssd_dt_bias,
        "ssd_a_log": ssd_a_log,
        "ssd_d": 1.0 + 0.1 * jax.random.normal(ks[9], (DEPTH, SSD_HEADS), f32),
        "ssd_norm_g": gain(ks[10], (DEPTH, SSD_INNER)),
        "attn_q_norm_g": gain(ks[11], (DEPTH, HEAD_DIM)),
        "attn_k_norm_g": gain(ks[12], (DEPTH, HEAD_DIM)),
        "mem_norm_g": gain(ks[13], (DEPTH, D_MODEL)),
        "w_mem_kv": w(ks[14], (DEPTH, D_MODEL, 2 * MEM_INNER), D_MODEL),
        "mem_q_norm_g": gain(ks[15], (DEPTH, HEAD_DIM)),
        "mem_k_norm_g": gain(ks[16], (DEPTH, HEAD_DIM)),
        "w_out": w(ks[17], (DEPTH, D_MIX, D_MODEL), D_MIX),
        "ffn_norm_g": gain(ks[18], (DEPTH, D_MODEL)),
        "w_up": w(ks[19], (DEPTH, D_MODEL, 2 * D_FF), D_MODEL),
        "ffn_conv_w": w(ks[20], (DEPTH, FFN_CONV, 2 * D_FF), FFN_CONV),
        "ffn_conv_b": 0.01 * jax.random.normal(ks[21], (DEPTH, 2 * D_FF), f32),
        "w_down": w(ks[22], (DEPTH, D_FF, D_MODEL), D_FF),
    }


def reference(x, mem, pos, mix_norm_g, w_in, ssd_conv_w, ssd_conv_b, ssd_dt_bias, ssd_a_log,
              ssd_d, ssd_norm_g, attn_q_norm_g, attn_k_norm_g, mem_norm_g, w_mem_kv,
              mem_q_norm_g, mem_k_norm_g, w_out, ffn_norm_g, w_up, ffn_conv_w, ffn_conv_b,
              w_down):
    for l in range(DEPTH):
        h = rmsnorm(x, mix_norm_g[l])
        proj = h @ w_in[l]
        z, xbc, dt, q, k, v, iq, ik, iw, mq = jnp.split(proj, IN_SPLITS, axis=-1)
        y_ssd = ssd_mixer(z, xbc, dt, ssd_conv_w[l], ssd_conv_b[l], ssd_dt_bias[l],
                          ssd_a_log[l], ssd_d[l], ssd_norm_g[l])
        y_dsa = dsa_mixer(q, k, v, iq, ik, iw, pos, attn_q_norm_g[l], attn_k_norm_g[l])
        mem_kv = rmsnorm(mem, mem_norm_g[l]) @ w_mem_kv[l]
        y_mem = mem_mixer(mq, mem_kv, mem_q_norm_g[l], mem_k_norm_g[l])
        y_mix = jnp.concatenate([y_ssd.astype(x.dtype), y_dsa.astype(x.dtype),
                                 y_mem.astype(x.dtype)], axis=-1)
        x = x + (y_mix @ w_out[l]).astype(x.dtype)
        x = x + conv_ffn(rmsnorm(x, ffn_norm_g[l]), w_up[l], ffn_conv_w[l], ffn_conv_b[l],
                         w_down[l]).astype(x.dtype)
    return x
```

```python
import numpy as np
import concourse.bass as bass
import concourse.mybir as mybir
from concourse.bass_utils import run_bass_kernel_spmd

F32 = mybir.dt.float32
BF16 = mybir.dt.bfloat16
I32 = mybir.dt.int32
FP8 = mybir.dt.int8
ALU = mybir.AluOpType
AF = mybir.ActivationFunctionType
AX = mybir.AxisListType

SAME_ENGINE_SYNC = True
N_DMA_SEMS = 24
SEM_SWITCH = 6000
NITER = 16
DBG_BLOCKS = None
DBG_BLOCKS_A = None
EPS = 1e-6

NL = 2
D = 1024
T = 4096
NT = T // 128
KC = D // 128
DFF = 2816
NFM = 1024
TM_Z, TM_IQ, TM_IK, TM_IK2, TM_Q, TM_K, TM_MQ, TM_V, TM_DT, TM_IW, NTM = 0, 512, 768, 832, 896, 1152, 1408, 1664, 1920, 1928, 1932
NIN = NFM + NTM
PP_MIXG, PP_FFNG, PP_MEMG, PP_SSDNG, PP_CONVW, PP_CONVB, PP_FCW, PP_FCB, NPP = 0, 8, 16, 24, 28, 60, 68, 200, 244
RP_DTB, RP_ALOG, RP_D, RP_QG, RP_KG, RP_MQG, RP_MKG, RP_CB, NRP = 0, 8, 16, 24, 88, 152, 216, 280, 1048
CC_ID, CC_U, CC_SL, CC_ONES, CC_NEG, CC_IF, CC_P2, NCC = 0, 128, 256, 384, 512, 2560, 2568, 2568 + 32


class Buf:
    __slots__ = ("w", "r", "x")

    def __init__(self, excl=False):
        self.w = None
        self.r = {}
        self.x = excl


class TL:
    __slots__ = ("ap", "b")

    def __init__(self, ap, b=None):
        if isinstance(ap, TL):
            self.ap, self.b = ap.ap, ap.b
            return
        self.ap = ap
        self.b = b if b is not None else Buf()

    def v(self, ap):
        return TL(ap, self.b)


def _bufs(xs):
    return [x.b if isinstance(x, TL) else x for x in xs]


class Prog:
    ENGS = ("pe", "act", "dve", "pool", "sp")

    def __init__(self, nc):
        self.nc = nc
        self.streams = {e: [] for e in self.ENGS}
        self.count = {e: 0 for e in self.ENGS}
        self.epoch = {e: 0 for e in self.ENGS}
        self.waited = {e: {} for e in self.ENGS}
        self.dma_val = [0] * N_DMA_SEMS
        self.dma_rr = 0
        self.out_events = []
        self.nops = 0
        self.cut = None
        self.marks = []

    def stage(self, name):
        self.marks.append((name, self.nops))

    def _collect(self, eng, reads, writes, extra=()):
        need = {}

        def add(ev):
            if ev is None:
                return
            k, v = ev
            if need.get(k, 0) < v:
                need[k] = v

        for b in reads:
            add(b.w)
            if b.x:
                for k, v in b.r.items():
                    if k != eng:
                        add((k, v))
        for b in writes:
            add(b.w)
            for k, v in b.r.items():
                add((k, v))
        for ev in extra:
            add(ev)
        waits = []
        for k, v in need.items():
            if k[0] == eng and (eng == "pe" or not SAME_ENGINE_SYNC):
                continue
            if self.waited[eng].get(k, 0) >= v:
                continue
            self.waited[eng][k] = v
            waits.append((k, v))
        return waits

    def _commit(self, ev, reads, writes):
        k, v = ev
        for b in reads:
            if b.r.get(k, 0) < v:
                b.r[k] = v
        for b in writes:
            b.w = ev
            b.r = {}

    def op(self, eng, fn, reads=(), writes=()):
        self.nops += 1
        if self.cut is not None and self.nops > self.cut:
            return None
        reads = _bufs(reads)
        writes = _bufs(writes)
        waits = self._collect(eng, reads, writes)
        self.count[eng] += 1
        key = (eng, self.epoch[eng])
        ev = (key, self.count[eng])
        self.streams[eng].append((waits, fn, key))
        self._commit(ev, reads, writes)
        return ev

    def dma(self, out_ap, in_ap, reads=(), writes=(), q="sp", is_output=False, **kw):
        self.nops += 1
        if self.cut is not None and self.nops > self.cut:
            return None
        reads = _bufs(reads)
        writes = _bufs(writes)
        s = self.dma_rr
        self.dma_rr = (self.dma_rr + 1) % N_DMA_SEMS
        key = ("dma", s)
        extra = [(key, self.dma_val[s])] if self.dma_val[s] > 0 else []
        waits = self._collect(q, reads, writes, extra)
        self.dma_val[s] += 16
        ev = (key, self.dma_val[s])
        self.streams[q].append((waits, lambda e: e.dma_start(out=out_ap, in_=in_ap, **kw), key))
        self._commit(ev, reads, writes)
        if is_output:
            self.out_events.append(ev)
        return ev

    def barrier(self):
        for e in self.ENGS:
            waits = []
            for k2 in self.ENGS:
                key = (k2, self.epoch[k2])
                if k2 != e and self.count[k2] > self.waited[e].get(key, 0):
                    self.waited[e][key] = self.count[k2]
                    waits.append((key, self.count[k2]))
            for s in range(N_DMA_SEMS):
                k = ("dma", s)
                if self.dma_val[s] > self.waited[e].get(k, 0):
                    self.waited[e][k] = self.dma_val[s]
                    waits.append((k, self.dma_val[s]))
            if waits:
                self.streams[e].append((waits, None, None))
        for e in self.ENGS:
            if self.count[e] > SEM_SWITCH:
                self.epoch[e] += 1
                self.count[e] = 0

    def emit(self):
        nc = self.nc
        from contextlib import ExitStack
        with ExitStack() as es:
            sems = {}
            for e in self.ENGS:
                for ep in range(self.epoch[e] + 1):
                    sems[(e, ep)] = es.enter_context(nc.semaphore("s_%s%d" % (e, ep)))
            for i in range(N_DMA_SEMS):
                sems[("dma", i)] = es.enter_context(nc.semaphore("s_dma%d" % i))
            block = es.enter_context(nc.Block())
            fin = {}
            for k, v in self.out_events:
                fin[k] = max(fin.get(k, 0), v)

            def run(name):
                def f(e):
                    for waits, fn, inc in self.streams[name]:
                        for k, v in waits:
                            e.wait_ge(sems[k], v)
                        if fn is None:
                            continue
                        ins = fn(e)
                        ins.then_inc(sems[inc], 16 if inc[0] == "dma" else 1)
                    if name == "sp":
                        for k, v in fin.items():
                            e.wait_ge(sems[k], v)
                return f

            block.sync(run("sp"))
            block.tensor(run("pe"))
            block.scalar(run("act"))
            block.vector(run("dve"))
            block.gpsimd(run("pool"))


class Arena:
    def __init__(self, ap_f32, nwords):
        self.ap = ap_f32
        self.n = nwords
        self.off = 0
        self.mark_ = 0
        self.live = []

    def mark(self):
        self.mark_ = self.off

    def reset(self):
        self.off = self.mark_

    def _alloc(self, w):
        o = self.off
        self.off += w
        assert self.off <= self.n, ("arena overflow", self.off, self.n)
        s_, e_ = o, o + w
        keep, inherit, same = [], [], None
        for (s2, e2, b2) in self.live:
            if e2 <= s_ or s2 >= e_:
                keep.append((s2, e2, b2))
            elif s2 == s_ and e2 == e_:
                same = b2
            else:
                inherit.append(b2)
        if same is not None and not inherit:
            buf = same
        else:
            buf = Buf()
            for b2 in inherit + ([same] if same is not None else []):
                evs = list(b2.r.items()) + ([b2.w] if b2.w is not None else [])
                for k, v in evs:
                    if buf.r.get(k, 0) < v:
                        buf.r[k] = v
        self.live = keep + [(s_, e_, buf)]
        return o, buf

    def f32(self, n):
        o, buf = self._alloc(n)
        return TL(self.ap[:, o:o + n], buf)

    def bf16(self, n):
        w = (n + 1) // 2
        o, buf = self._alloc(w)
        return TL(self.ap[:, o:o + w].bitcast(BF16)[:, 0:n], buf)

    def fp8(self, n):
        w = (n + 3) // 4
        o, buf = self._alloc(w)
        return TL(self.ap[:, o:o + w].bitcast(FP8)[:, 0:n], buf)

    def i32(self, n):
        t = self.f32(n)
        return TL(t.ap.bitcast(I32), t.b)


DBG_CUT = None
LAST_PROG = [None]


def build(layers=(0, 1), upto="all", debug=()):
    nc = bass.Bass("TRN2", target_bir_lowering=False)
    P = Prog(nc)
    P.cut = DBG_CUT
    LAST_PROG[0] = P

    def din(name, shape, dt=F32):
        return nc.dram_tensor(name, list(shape), dt, kind="ExternalInput").ap()

    def dscr(name, shape, dt):
        kind = "ExternalOutput" if name in debug else "Internal"
        return nc.dram_tensor(name, list(shape), dt, kind=kind).ap()

    xT_d = din("xT", [D, T])
    memT_d = din("memT", [D, 256])
    pos_d = din("posr", [128, NT], I32)
    win_d = din("w_in_r", [NL, D, NIN])
    wkv_d = din("w_mem_kv", [NL, D, 512])
    wout_d = din("w_out", [NL, D, D])
    wup_d = din("w_up", [NL, D, 2 * DFF])
    wdn_d = din("w_down", [NL, DFF, D])
    pp_d = din("pp", [NL, 128, NPP])
    rp_d = din("rowp", [NL, NRP])
    cc_d = din("consts", [128, NCC])
    yT_d = nc.dram_tensor("yT", [D, T], F32, kind="ExternalOutput").ap()

    FMT_D = dscr("FMT", [9, 128, T], BF16)
    VP_D = dscr("VP", [128, NT, 264], BF16)
    WI_D = dscr("WI", [128, NT, 4], F32)
    YM_D = dscr("YMIXT", [D, T], BF16)
    X1_D = dscr("X1T", [D, T], F32)
    X2_D = dscr("X2T", [D, T], F32)
    b_FMT, b_VP, b_WI, b_YM, b_X1, b_X2, b_Y = Buf(), Buf(), Buf(), Buf(), Buf(), Buf(), Buf()

    ARW = 50 * 1024
    arena_t = nc.alloc_sbuf_tensor("arena", [128, ARW], F32).ap()
    A = Arena(arena_t, ARW)
    banks = [TL(nc.alloc_psum_tensor("bank%d" % i, [128, 512], F32).ap(), Buf(excl=True)) for i in range(8)]
    rot = {"i": 0, "set": list(range(8))}

    def nb():
        s = rot["set"]
        b = banks[s[rot["i"] % len(s)]]
        rot["i"] += 1
        return b

    def mm(out, lhsT, rhs, start=True, stop=True, reads=(), writes=()):
        return P.op("pe", lambda e: e.matmul(out, lhsT, rhs, start=start, stop=stop), reads, writes)

    def tr(out, in_, ident, reads=(), writes=()):
        return P.op("pe", lambda e: e.transpose(out, in_, ident), reads, writes)

    def act(out, in_, func, reads=(), writes=(), **kw):
        return P.op("act", lambda e: e.activation(out, in_, func, **kw), reads, writes)

    def tt(eng, out, in0, in1, op, reads=(), writes=()):
        return P.op(eng, lambda e: e.tensor_tensor(out, in0, in1, op), reads, writes)

    def ts(eng, out, in0, s1, s2, op0, op1=None, reads=(), writes=(), accum_out=None):
        if accum_out is not None:
            return P.op(eng, lambda e: e.tensor_scalar(out, in0, s1, s2, op0, op1, accum_out=accum_out), reads, writes)
        if op1 is None:
            return P.op(eng, lambda e: e.tensor_scalar(out, in0, s1, None, op0), reads, writes)
        return P.op(eng, lambda e: e.tensor_scalar(out, in0, s1, s2, op0, op1), reads, writes)

    def stt(eng, out, in0, scalar, in1, op0, op1, reads=(), writes=()):
        return P.op(eng, lambda e: e.scalar_tensor_tensor(out, in0, scalar, in1, op0, op1), reads, writes)

    def cp(eng, out, in_, reads=(), writes=()):
        if eng == "act":
            return P.op("act", lambda e: e.activation(out, in_, AF.Copy), reads, writes)
        return P.op(eng, lambda e: e.tensor_copy(out, in_), reads, writes)

    def ms(eng, ap, val, writes=()):
        return P.op(eng, lambda e: e.memset(ap, val), (), writes)

    def red(eng, out, in_, op, reads=(), writes=(), **kw):
        return P.op(eng, lambda e: e.tensor_reduce(out, in_, AX.X, op, **kw), reads, writes)

    def bc(ap, axis, shape):
        return ap.unsqueeze(axis).to_broadcast(list(shape))

    rr = {"i": 0}

    def evac_eng():
        rr["i"] += 1
        return ("act", "dve")[rr["i"] % 2]

    CC = TL(A.f32(NCC))
    P.dma(CC.ap, cc_d, writes=[CC])
    IDENT32 = CC.ap[:, CC_ID:CC_ID + 128]
    U32 = CC.ap[:, CC_U:CC_U + 128]
    SL32 = CC.ap[:, CC_SL:CC_SL + 128]
    ONES32 = CC.ap[:, CC_ONES:CC_ONES + 128]
    NEGM = CC.ap[:, CC_NEG:CC_NEG + 2048].rearrange("p (a b) -> p a b", a=4)
    INVF = CC.ap[:, CC_IF:CC_IF + 8]
    POW2 = CC.ap[:, CC_P2:CC_P2 + 32]
    CB16 = TL(A.bf16(256))
    IDB = CB16.ap[:, 0:128]
    ONESB = CB16.ap[:, 128:256]
    cp("dve", IDB, IDENT32, [CC], [CB16])
    cp("dve", ONESB, ONES32, [CC], [CB16])
    ROW16 = TL(A.bf16(128 + 768))
    ONESROW = ROW16.ap[0:1, 0:128]
    BIASROW = ROW16.ap[0:1, 128:896]
    EPSC = TL(A.f32(1))
    ms("pool", EPSC.ap, EPS, [EPSC])
    MH = TL(A.f32(16))
    ms("pool", MH.ap, -0.5, [MH])
    PPs, RPs = [], []
    for l in range(NL):
        t = TL(A.f32(NPP))
        P.dma(t.ap, pp_d[l], writes=[t])
        PPs.append(t)
        t = TL(A.f32(NRP))
        P.dma(t.ap, rp_d[l].partition_broadcast(128), writes=[t])
        RPs.append(t)
    COS = TL(A.f32(NT * 8))
    SIN = TL(A.f32(NT * 8))
    A.mark()
    BASE = A.off
    if True:
        PI_ = TL(A.i32(NT))
        PF_ = TL(A.f32(NT))
        P.dma(PI_.ap, pos_d, writes=[PI_])
        cp("dve", PF_.ap, PI_.ap, [PI_], [PF_])
        ANG = TL(A.f32(NT * 8))
        KF = TL(A.f32(NT * 8))
        KI = TL(A.i32(NT * 8))
        ang3 = ANG.ap.rearrange("p (a b) -> p a b", a=NT)
        tt("dve", ang3, bc(PF_.ap, 2, [128, NT, 8]), bc(INVF, 1, [128, NT, 8]), ALU.mult, [PF_, CC], [ANG])
        TWO_PI = float(2 * np.pi)
        for dst, shift in ((SIN, 0.0), (COS, float(np.pi / 2))):
            ts("dve", dst.ap, ANG.ap, shift, None, ALU.add, None, [ANG], [dst])
            ts("dve", KF.ap, dst.ap, 1.0 / TWO_PI, None, ALU.mult, None, [dst], [KF])
            cp("dve", KI.ap, KF.ap, [KF], [KI])
            cp("dve", KF.ap, KI.ap, [KI], [KF])
            stt("dve", dst.ap, KF.ap, -6.28125, dst.ap, ALU.mult, ALU.add, [KF, dst], [dst])
            stt("dve", dst.ap, KF.ap, float(-(TWO_PI - 6.28125)), dst.ap, ALU.mult, ALU.add, [KF, dst], [dst])
            ts("dve", KF.ap, dst.ap, float(np.pi), -TWO_PI, ALU.is_gt, ALU.mult, [dst], [KF])
            tt("dve", dst.ap, dst.ap, KF.ap, ALU.add, [dst, KF], [dst])
            ts("dve", KF.ap, dst.ap, float(-np.pi), TWO_PI, ALU.is_lt, ALU.mult, [dst], [KF])
            tt("dve", dst.ap, dst.ap, KF.ap, ALU.add, [dst, KF], [dst])
            ts("dve", dst.ap, dst.ap, float(np.pi), float(-np.pi), ALU.min, ALU.max, [dst], [dst])
            act(dst.ap, dst.ap, AF.Sin, [dst], [dst])
    P.stage("consts_done")
    COS3 = COS.ap.rearrange("p (a b) -> p a b", a=NT)
    SIN3 = SIN.ap.rearrange("p (a b) -> p a b", a=NT)

    wl_state = {}

    def load_weight(dst_tl, dst3, src2d, K, N, scale_fn):
        CH = 2048
        if wl_state.get("end") != A.off:
            wl_state["stg"] = [TL(A.f32(CH)) for _ in range(3)]
            wl_state["end"] = A.off
        stg = wl_state["stg"]
        i = 0
        engs = ("act", "dve", "pool")
        for c in range(K // 128):
            for n0 in range(0, N, CH):
                n1 = min(N, n0 + CH)
                st = stg[i % 3]
                P.dma(st.ap[:, 0:n1 - n0], src2d[c * 128:(c + 1) * 128, n0:n1], writes=[st])
                sc = scale_fn(c) if scale_fn is not None else None
                eng = engs[i % 3]
                o = dst3[:, c, n0:n1]
                s_ap = st.ap[:, 0:n1 - n0]
                rd = [st] + ([sc[1]] if sc is not None else [])
                if sc is None:
                    cp(eng, o, s_ap, rd, [dst_tl])
                elif eng == "act":
                    act(o, s_ap, AF.Copy, rd, [dst_tl], scale=sc[0])
                elif eng == "dve":
                    ts("dve", o, s_ap, sc[0], None, ALU.mult, None, rd, [dst_tl])
                else:
                    ts("pool", o, s_ap, sc[0], 1.0, ALU.mult, ALU.mult, rd, [dst_tl])
                i += 1

    def rmsnorm_block(XB, HT, ncols):
        SQ = TL(A.bf16(KC * ncols))
        sq3 = SQ.ap.rearrange("p (c t) -> p c t", c=KC)
        act(sq3, XB.ap, AF.Square, [XB], [SQ])
        bk = nb()
        for c in range(KC):
            mm(bk.ap[:, 0:ncols], ONESB, sq3[:, c, :], c == 0, c == KC - 1, [CB16, SQ], [bk])
        RS = TL(A.f32(ncols))
        act(RS.ap, bk.ap[:, 0:ncols], AF.Ln, [bk, EPSC], [RS], bias=EPSC.ap, scale=1.0 / D)
        act(RS.ap, RS.ap, AF.Exp, [RS], [RS], scale=-0.5)
        tt("dve", HT.ap, XB.ap, bc(RS.ap, 1, [128, KC, ncols]), ALU.mult, [XB, RS], [HT])

    def phase_A(l, x_src, b_xsrc):
        PP, RP = PPs[l], RPs[l]
        P.barrier()
        A.off = BASE
        wl_state.clear()
        WIN = TL(A.bf16(KC * NIN))
        win3 = WIN.ap.rearrange("p (c n) -> p c n", c=KC)
        a0 = A.off
        load_weight(WIN, win3, win_d[l], D, NIN, lambda c: (PP.ap[:, PP_MIXG + c:PP_MIXG + c + 1], PP))
        P.barrier()
        A.off = a0
        P.stage("A_weights_loaded")
        DIAGW = TL(A.bf16(32 * 128))
        dg3 = DIAGW.ap.rearrange("p (a b) -> p a b", a=32)
        for j in range(4):
            for c in range(8):
                ts("pool", dg3[:, j * 8 + c, :], IDB, PP.ap[:, PP_CONVW + j * 8 + c:PP_CONVW + j * 8 + c + 1], 1.0,
                   ALU.mult, ALU.mult, [CB16, PP], [DIAGW])
        DI = TL(A.bf16(8 * 128))
        di3 = DI.ap.rearrange("p (a b) -> p a b", a=8)
        for h in range(8):
            ts("pool", di3[:, h, :], IDB, RP.ap[:, RP_D + h:RP_D + h + 1], 1.0, ALU.mult, ALU.mult, [CB16, RP], [DI])
        ANEG = TL(A.f32(8))
        act(ANEG.ap, RP.ap[:, RP_ALOG:RP_ALOG + 8], AF.Exp, [RP], [ANEG])
        ts("dve", ANEG.ap, ANEG.ap, -1.0, None, ALU.mult, None, [ANEG], [ANEG])
        GN = TL(A.f32(12 * 64))
        gn3 = GN.ap.rearrange("p (a b) -> p a b", a=12)
        for i, off in enumerate((RP_QG, RP_KG, RP_MQG)):
            cp("dve", gn3[:, i * 4:(i + 1) * 4, :], bc(RP.ap[:, off:off + 64], 1, [128, 4, 64]), [RP], [GN])
        ms("pool", ONESROW, 1.0, [ROW16])
        cp("dve", BIASROW, RP.ap[0:1, RP_CB:RP_CB + 768], [RP], [ROW16])
        DTB = RP.ap[:, RP_DTB:RP_DTB + 8]
        H = TL(A.f32(512))
        HB = TL(A.bf16(512))
        ms("pool", H.ap, 0.0, [H])
        ms("pool", HB.ap, 0.0, [HB])
        RAW = TL(A.bf16(8 * 516))
        raw3 = RAW.ap.rearrange("p (c t) -> p c t", c=8)
        ms("pool", RAW.ap, 0.0, [RAW])
        VPB = [TL(A.bf16(4 * 264)) for _ in range(2)]
        for t_ in VPB:
            ms("pool", t_.ap, 1.0, [t_])
        A.mark()
        XSL = [TL(A.bf16(512)) for _ in range(2)]
        BTML = [TL(A.bf16(256)) for _ in range(2)]
        ZSL = [TL(A.bf16(512)) for _ in range(2)]
        DTL = [TL(A.f32(8)) for _ in range(2)]
        ADTL = [TL(A.f32(8)) for _ in range(2)]
        h3v = H.ap.rearrange("p (h d) -> p h d", h=8)
        P.stage("A_layer_consts")

        for blk in range(DBG_BLOCKS_A or DBG_BLOCKS or T // 512):
            A.reset()
            c0 = blk * 512
            XB = TL(A.f32(KC * 512))
            xb3 = XB.ap.rearrange("p (c t) -> p c t", c=KC)
            P.dma(xb3, x_src[:, c0:c0 + 512].rearrange("(c p) t -> p c t", p=128), reads=[b_xsrc], writes=[XB])
            HT = TL(A.bf16(KC * 512))
            HT.ap = HT.ap.rearrange("p (c t) -> p c t", c=KC)
            XB3 = TL(xb3, XB.b)
            rmsnorm_block(XB3, HT, 512)
            ht3 = HT.ap
            P.stage("A_blk%d_norm" % blk)
            for c8 in range(8):
                bk = nb()
                for k in range(KC):
                    mm(bk.ap, win3[:, k, c8 * 128:(c8 + 1) * 128], ht3[:, k, :], k == 0, k == KC - 1, [WIN, HT], [bk])
                cp(evac_eng(), raw3[:, c8, 3:515], bk.ap, [bk], [RAW])
            P.stage("A_blk%d_fmproj" % blk)
            BCT = TL(A.bf16(4 * 512))
            bct3 = BCT.ap.rearrange("p (c t) -> p c t", c=4)
            for j4 in range(4):
                bk = nb()
                for tap in range(4):
                    mm(bk.ap, dg3[:, tap * 8 + 4 + j4, :], raw3[:, 4 + j4, tap:tap + 512], tap == 0, tap == 3, [DIAGW, RAW], [bk])
                act(bct3[:, j4, :], bk.ap, AF.Silu, [bk, PP], [BCT], bias=PP.ap[:, PP_CONVB + 4 + j4:PP_CONVB + 5 + j4])
            P.stage("A_blk%d_bcconv" % blk)
            FMS = TL(A.bf16(9 * 512))
            fms3 = FMS.ap.rearrange("p (c t) -> p c t", c=9)
            YST = TL(A.bf16(4 * 512))
            yst3 = YST.ap.rearrange("p (c t) -> p c t", c=4)
            WIB = TL(A.f32(16))
            wib3 = WIB.ap.rearrange("p (a b) -> p a b", a=4)
            vpb = VPB[blk % 2]
            vpb4 = vpb.ap.rearrange("p (t h d) -> p t h d", t=4, h=4)
            a_tile = A.off
            tstate = {}

            def front(ti):
                    A.off = a_tile
                    o = ti * 128
                    g = blk * 4 + ti
                    bk = nb()
                    for c4 in range(4):
                        for tap in range(4):
                            mm(bk.ap[:, c4 * 128:(c4 + 1) * 128], raw3[:, c4, o + tap:o + tap + 128], dg3[:, tap * 8 + c4, :],
                               tap == 0, False, [RAW, DIAGW], [bk])
                        mm(bk.ap[:, c4 * 128:(c4 + 1) * 128], ONESROW, BIASROW[:, c4 * 128:(c4 + 1) * 128], False, True, [ROW16], [bk])
                    XS = XSL[g % 2]
                    act(XS.ap, bk.ap, AF.Silu, [bk], [XS])
                    bk = nb()
                    for c2 in range(2):
                        for tap in range(4):
                            mm(bk.ap[:, c2 * 128:(c2 + 1) * 128], raw3[:, 4 + c2, o + tap:o + tap + 128], dg3[:, tap * 8 + 4 + c2, :],
                               tap == 0, False, [RAW, DIAGW], [bk])
                        mm(bk.ap[:, c2 * 128:(c2 + 1) * 128], ONESROW, BIASROW[:, 512 + c2 * 128:512 + (c2 + 1) * 128], False, True, [ROW16], [bk])
                    BTM = BTML[g % 2]
                    act(BTM.ap, bk.ap[:, 0:256], AF.Silu, [bk], [BTM])
                    P.stage("A_t%d_xsconv" % g)
                    TMX = TL(A.f32(18 * 64))
                    tmx3 = TMX.ap.rearrange("p (h d) -> p h d", h=18)
                    ZS = ZSL[g % 2]
                    DT = DTL[g % 2]
                    groups = ((TM_Z, 512), (TM_IQ, 384), (TM_Q, 512), (TM_MQ, 512), (TM_DT, 12))
                    for gi, (col0, ncol) in enumerate(groups):
                        bk = nb()
                        for k in range(KC):
                            mm(bk.ap[:, 0:ncol], ht3[:, k, o:o + 128], win3[:, k, NFM + col0:NFM + col0 + ncol], k == 0, k == KC - 1, [HT, WIN], [bk])
                        if gi == 0:
                            act(ZS.ap, bk.ap, AF.Silu, [bk], [ZS])
                        elif gi == 1:
                            cp("act", TMX.ap[:, 0:384], bk.ap[:, 0:384], [bk], [TMX])
                        elif gi == 2:
                            cp("dve", TMX.ap[:, 384:896], bk.ap, [bk], [TMX])
                        elif gi == 3:
                            cp("act", TMX.ap[:, 896:1152], bk.ap[:, 0:256], [bk], [TMX])
                            cp("act", vpb4[:, ti, :, 0:64], bk.ap[:, 256:512].rearrange("p (h d) -> p h d", h=4), [bk], [vpb])
                        else:
                            tt("dve", DT.ap, bk.ap[:, 0:8], DTB, ALU.add, [bk, RP], [DT])
                            ts("dve", wib3[:, ti, :], bk.ap[:, 8:12], 1.0 / 16.0, None, ALU.mult, None, [bk], [WIB])
                    P.stage("A_t%d_tmproj" % g)
                    act(DT.ap, DT.ap, AF.Exp, [DT], [DT])
                    act(DT.ap, DT.ap, AF.Ln, [DT], [DT], bias=1.0)
                    ADT = ADTL[g % 2]
                    tt("dve", ADT.ap, DT.ap, ANEG.ap, ALU.mult, [DT, ANEG], [ADT])
                    P.stage("A_t%d_softplus" % g)
                    SQN = TL(A.f32(12 * 64))
                    sqn3 = SQN.ap.rearrange("p (h d) -> p h d", h=12)
                    SS = TL(A.f32(12))
                    tt("pool", sqn3, tmx3[:, 6:18, :], tmx3[:, 6:18, :], ALU.mult, [TMX], [SQN])
                    red("dve", SS.ap, sqn3, ALU.add, [SQN], [SS])
                    ts("dve", SS.ap, SS.ap, 1.0 / 64.0, EPS, ALU.mult, ALU.add, [SS], [SS])
                    tt("pool", SS.ap, SS.ap, MH.ap[:, 0:12], ALU.pow, [SS, MH], [SS])
                    tt("dve", tmx3[:, 6:18, :], tmx3[:, 6:18, :], bc(SS.ap, 2, [128, 12, 64]), ALU.mult, [TMX, SS], [TMX])
                    tt("dve", tmx3[:, 6:18, :], tmx3[:, 6:18, :], gn3, ALU.mult, [TMX, GN], [TMX])
                    P.stage("A_t%d_norm" % g)
                    TMB = TL(A.bf16(18 * 64))
                    tmb3 = TMB.ap.rearrange("p (h d) -> p h d", h=18)
                    cp("act", TMB.ap, TMX.ap, [TMX], [TMB])
                    x1 = tmx3[:, 0:14, 0:8]
                    x2 = tmx3[:, 0:14, 8:16]
                    cosb = bc(COS3[:, g, :], 1, [128, 14, 8])
                    sinb = bc(SIN3[:, g, :], 1, [128, 14, 8])
                    RT = [TL(A.f32(112)) for _ in range(4)]
                    rt3 = [t_.ap.rearrange("p (h d) -> p h d", h=14) for t_ in RT]
                    tt("dve", rt3[0], x1, cosb, ALU.mult, [TMX, COS], [RT[0]])
                    tt("pool", rt3[1], x2, sinb, ALU.mult, [TMX, SIN], [RT[1]])
                    tt("dve", rt3[2], x2, cosb, ALU.mult, [TMX, COS], [RT[2]])
                    tt("pool", rt3[3], x1, sinb, ALU.mult, [TMX, SIN], [RT[3]])
                    tt("dve", tmb3[:, 0:14, 0:8], rt3[0], rt3[1], ALU.subtract, [RT[0], RT[1]], [TMB])
                    tt("dve", tmb3[:, 0:14, 8:16], rt3[2], rt3[3], ALU.add, [RT[2], RT[3]], [TMB])
                    P.stage("A_t%d_rope" % g)
                    bkA, bkB = nb(), nb()
                    tA = bkA.ap.bitcast(BF16)
                    tB = bkB.ap.bitcast(BF16)
                    for i in range(9):
                        dst = tA[:, i * 128:(i + 1) * 128] if i < 5 else tB[:, (i - 5) * 128:(i - 4) * 128]
                        tr(dst, TMB.ap[:, i * 128:(i + 1) * 128], IDB, [TMB, CB16], [bkA if i < 5 else bkB])
                    cp("act", fms3[:, 0:5, o:o + 128], tA[:, 0:640].rearrange("p (a b) -> p a b", a=5), [bkA], [FMS])
                    cp("dve", fms3[:, 5:9, o:o + 128], tB[:, 0:512].rearrange("p (a b) -> p a b", a=4), [bkB], [FMS])

                    P.stage("A_t%d_transp" % g)
                    tstate[ti] = (XS, BTM, ZS, DT, ADT)
                    if "a_back" not in tstate:
                        tstate["a_back"] = A.off

            def back(ti):
                    A.off = tstate["a_back"]
                    o = ti * 128
                    g = blk * 4 + ti
                    XS, BTM, ZS, DT, ADT = tstate.pop(ti)
                    xs3 = XS.ap.rearrange("p (h d) -> p h d", h=8)
                    bk_cs = nb()
                    mm(bk_cs.ap[:, 0:8], U32, ADT.ap, True, True, [CC, ADT], [bk_cs])
                    mm(bk_cs.ap[:, 8:16], ONES32, ADT.ap, True, True, [CC, ADT], [bk_cs])
                    CST = TL(A.f32(24))
                    cp("act", CST.ap[:, 0:16], bk_cs.ap[:, 0:16], [bk_cs], [CST])
                    tt("dve", CST.ap[:, 16:24], CST.ap[:, 8:16], CST.ap[:, 0:8], ALU.subtract, [CST], [CST])
                    ECS = TL(A.f32(24))
                    act(ECS.ap, CST.ap, AF.Exp, [CST], [ECS])
                    AH = TL(A.f32(8 * 128))
                    ah3 = AH.ap.rearrange("p (h s) -> p h s", h=8)
                    tt("dve", ah3, bc(SL32, 1, [128, 8, 128]), bc(ADT.ap, 2, [128, 8, 128]), ALU.mult, [CC, ADT], [AH])
                    E = TL(A.f32(1024))
                    for g2 in range(2):
                        bk = nb()
                        for r in range(4):
                            mm(bk.ap[:, r * 128:(r + 1) * 128], ah3[:, g2 * 4 + r, :], U32, True, True, [AH, CC], [bk])
                        act(E.ap[:, g2 * 512:(g2 + 1) * 512], bk.ap, AF.Exp, [bk], [E])
                    P.stage("A_t%d_ssdE" % g)
                    bk_cb = nb()
                    for g2 in range(2):
                        mm(bk_cb.ap[:, g2 * 128:(g2 + 1) * 128], bct3[:, g2, o:o + 128], bct3[:, 2 + g2, o:o + 128], True, True, [BCT], [bk_cb])
                    CBM = TL(A.f32(256))
                    cbm3 = CBM.ap.rearrange("p (g l) -> p g l", g=2)
                    tt("dve", cbm3, bk_cb.ap[:, 0:256].rearrange("p (g l) -> p g l", g=2), bc(U32, 1, [128, 2, 128]), ALU.mult, [bk_cb, CC], [CBM])
                    MT = TL(A.bf16(1024))
                    mt3 = MT.ap.rearrange("p (h l) -> p h l", h=8)
                    tt("dve", MT.ap.rearrange("p (g r l) -> p g r l", g=2, r=4), E.ap.rearrange("p (g r l) -> p g r l", g=2, r=4),
                       bc(cbm3, 2, [128, 2, 4, 128]), ALU.mult, [E, CBM], [MT])
                    XDT = TL(A.bf16(512))
                    xdt3 = XDT.ap.rearrange("p (h d) -> p h d", h=8)
                    tt("pool", xdt3, xs3, bc(DT.ap, 2, [128, 8, 64]), ALU.mult, [XS, DT], [XDT])
                    XDEC = TL(A.bf16(512))
                    tt("pool", XDEC.ap.rearrange("p (h d) -> p h d", h=8), xdt3, bc(ECS.ap[:, 16:24], 2, [128, 8, 64]), ALU.mult, [XDT, ECS], [XDEC])
                    P.stage("A_t%d_ssdM" % g)
                    bk_st = nb()
                    for g2 in range(2):
                        mm(bk_st.ap[:, g2 * 256:(g2 + 1) * 256], BTM.ap[:, g2 * 128:(g2 + 1) * 128], XDEC.ap[:, g2 * 256:(g2 + 1) * 256],
                           True, True, [BTM, XDEC], [bk_st])
                    bk_y = nb()
                    for h in range(8):
                        mm(bk_y.ap[:, h * 64:(h + 1) * 64], mt3[:, h, :], xdt3[:, h, :], True, False, [MT, XDT], [bk_y])
                        mm(bk_y.ap[:, h * 64:(h + 1) * 64], di3[:, h, :], xs3[:, h, :], False, True, [DI, XS], [bk_y])
                    bk_yo = nb()
                    for g2 in range(2):
                        mm(bk_yo.ap[:, g2 * 256:(g2 + 1) * 256], bct3[:, 2 + g2, o:o + 128], HB.ap[:, g2 * 256:(g2 + 1) * 256],
                           True, True, [BCT, HB], [bk_yo])
                    Y = TL(A.f32(512))
                    y3 = Y.ap.rearrange("p (h d) -> p h d", h=8)
                    tt("dve", y3, bk_yo.ap.rearrange("p (h d) -> p h d", h=8), bc(ECS.ap[:, 0:8], 2, [128, 8, 64]), ALU.mult, [bk_yo, ECS], [Y])
                    tt("dve", Y.ap, Y.ap, bk_y.ap, ALU.add, [Y, bk_y], [Y])
                    P.stage("A_t%d_ssdY" % g)
                    tt("pool", h3v, h3v, bc(ECS.ap[:, 8:16], 2, [128, 8, 64]), ALU.mult, [H, ECS], [H])
                    tt("dve", H.ap, H.ap, bk_st.ap, ALU.add, [H, bk_st], [H])
                    cp("act", HB.ap, H.ap, [H], [HB])
                    P.stage("A_t%d_ssdH" % g)
                    YG = TL(A.f32(512))
                    tt("dve", YG.ap, Y.ap, ZS.ap, ALU.mult, [Y, ZS], [YG])
                    SSG = TL(A.f32(2))
                    JK = TL(A.bf16(256))
                    for g2 in range(2):
                        act(JK.ap, YG.ap[:, g2 * 256:(g2 + 1) * 256], AF.Square, [YG], [JK, SSG], accum_out=SSG.ap[:, g2:g2 + 1])
                    ts("dve", SSG.ap, SSG.ap, 1.0 / 256.0, EPS, ALU.mult, ALU.add, [SSG], [SSG])
                    tt("pool", SSG.ap, SSG.ap, MH.ap[:, 0:2], ALU.pow, [SSG, MH], [SSG])
                    YN = TL(A.bf16(512))
                    tt("dve", YN.ap.rearrange("p (g d) -> p g d", g=2), YG.ap.rearrange("p (g d) -> p g d", g=2),
                       bc(SSG.ap, 2, [128, 2, 256]), ALU.mult, [YG, SSG], [YN])
                    bkT = nb()
                    tT = bkT.ap.bitcast(BF16)
                    for c4 in range(4):
                        tr(tT[:, c4 * 128:(c4 + 1) * 128], YN.ap[:, c4 * 128:(c4 + 1) * 128], IDB, [YN, CB16], [bkT])
                    cp("act", yst3[:, :, o:o + 128], tT[:, 0:512].rearrange("p (a b) -> p a b", a=4), [bkT], [YST])

            front(0)
            for ti in range(4):
                if ti + 1 < 4:
                    front(ti + 1)
                back(ti)
            cp("pool", raw3[:, :, 0:3], raw3[:, :, 512:515], [RAW], [RAW])
            P.dma(FMT_D[:, :, c0:c0 + 512].rearrange("c p t -> p c t"), fms3, reads=[FMS], writes=[b_FMT])
            P.dma(YM_D[0:512, c0:c0 + 512].rearrange("(c p) t -> p c t", p=128), yst3, reads=[YST], writes=[b_YM])
            P.dma(VP_D[:, blk * 4:(blk + 1) * 4, :], vpb.ap.rearrange("p (t n) -> p t n", t=4), reads=[vpb], writes=[b_VP])
            P.dma(WI_D[:, blk * 4:(blk + 1) * 4, :], wib3, reads=[WIB], writes=[b_WI])

    def attn_tmp():
        return {"YA": [TL(A.bf16(2 * 512)) for _ in range(2)], "O": [TL(A.f32(260)) for _ in range(2)],
                "RD": [TL(A.f32(4)) for _ in range(2)], "YB": [TL(A.bf16(256)) for _ in range(2)], "n": 0}

    def attn_finish(accs, row0, c0, tmp):
        YA = tmp["YA"][tmp["n"] % 2]
        tmp["n"] += 1
        ya3 = YA.ap.rearrange("p (c t) -> p c t", c=2)
        for qt in range(4):
            O, RD, YB = tmp["O"][qt % 2], tmp["RD"][qt % 2], tmp["YB"][qt % 2]
            o3 = O.ap.rearrange("p (h d) -> p h d", h=4)
            cp("act", O.ap, accs[qt].ap[:, 0:260], [accs[qt]], [O])
            P.op("dve", lambda e, RD=RD, o3=o3: e.reciprocal(RD.ap, o3[:, :, 64]), [O], [RD])
            tt("dve", YB.ap.rearrange("p (h d) -> p h d", h=4), o3[:, :, 0:64], bc(RD.ap, 2, [128, 4, 64]), ALU.mult, [O, RD], [YB])
            bkT = nb()
            tT = bkT.ap.bitcast(BF16)
            for c2 in range(2):
                tr(tT[:, c2 * 128:(c2 + 1) * 128], YB.ap[:, c2 * 128:(c2 + 1) * 128], IDB, [YB, CB16], [bkT])
            cp("act", ya3[:, :, qt * 128:(qt + 1) * 128], tT[:, 0:256].rearrange("p (a b) -> p a b", a=2), [bkT], [YA])
        P.dma(YM_D[row0:row0 + 256, c0:c0 + 512].rearrange("(c p) t -> p c t", p=128), ya3, reads=[YA], writes=[b_YM])

    def phase_mem(l):
        PP, RP = PPs[l], RPs[l]
        P.barrier()
        A.off = BASE
        wl_state.clear()
        rot["set"] = [4, 5, 6, 7]
        accs = banks[0:4]
        WKV = TL(A.bf16(KC * 512))
        wkv3 = WKV.ap.rearrange("p (c n) -> p c n", c=KC)
        a0 = A.off
        load_weight(WKV, wkv3, wkv_d[l], D, 512, lambda c: (PP.ap[:, PP_MEMG + c:PP_MEMG + c + 1], PP))
        P.barrier()
        A.off = a0
        MB_ = TL(A.f32(KC * 256))
        mb3 = MB_.ap.rearrange("p (c t) -> p c t", c=KC)
        P.dma(mb3, memT_d.rearrange("(c p) t -> p c t", p=128), writes=[MB_])
        MN = TL(A.bf16(KC * 256))
        MN.ap = MN.ap.rearrange("p (c t) -> p c t", c=KC)
        rmsnorm_block(TL(mb3, MB_.b), MN, 256)
        MKT = TL(A.bf16(2 * 256))
        mkt3 = MKT.ap.rearrange("p (c m) -> p c m", c=2)
        MV = TL(A.bf16(2 * 264))
        ms("pool", MV.ap, 1.0, [MV])
        mv4 = MV.ap.rearrange("p (t h d) -> p t h d", t=2, h=4)
        gk = TL(A.f32(256))
        cp("dve", gk.ap.rearrange("p (h d) -> p h d", h=4), bc(RP.ap[:, RP_MKG:RP_MKG + 64], 1, [128, 4, 64]), [RP], [gk])
        for mt in range(2):
            bk = nb()
            for k in range(KC):
                mm(bk.ap, MN.ap[:, k, mt * 128:(mt + 1) * 128], wkv3[:, k, :], k == 0, k == KC - 1, [MN, WKV], [bk])
            MK = TL(A.f32(256))
            mk3 = MK.ap.rearrange("p (h d) -> p h d", h=4)
            cp("act", MK.ap, bk.ap[:, 0:256], [bk], [MK])
            cp("dve", mv4[:, mt, :, 0:64], bk.ap[:, 256:512].rearrange("p (h d) -> p h d", h=4), [bk], [MV])
            SQ = TL(A.f32(256))
            SS = TL(A.f32(4))
            tt("dve", SQ.ap, MK.ap, MK.ap, ALU.mult, [MK], [SQ])
            red("dve", SS.ap, SQ.ap.rearrange("p (h d) -> p h d", h=4), ALU.add, [SQ], [SS])
            ts("dve", SS.ap, SS.ap, 1.0 / 64.0, EPS, ALU.mult, ALU.add, [SS], [SS])
            tt("pool", SS.ap, SS.ap, MH.ap[:, 0:4], ALU.pow, [SS, MH], [SS])
            tt("dve", mk3, mk3, bc(SS.ap, 2, [128, 4, 64]), ALU.mult, [MK, SS], [MK])
            MKB = TL(A.bf16(256))
            tt("dve", MKB.ap, MK.ap, gk.ap, ALU.mult, [MK, gk], [MKB])
            bkT = nb()
            tT = bkT.ap.bitcast(BF16)
            for c2 in range(2):
                tr(tT[:, c2 * 128:(c2 + 1) * 128], MKB.ap[:, c2 * 128:(c2 + 1) * 128], IDB, [MKB, CB16], [bkT])
            cp("act", mkt3[:, :, mt * 128:(mt + 1) * 128], tT[:, 0:256].rearrange("p (a b) -> p a b", a=2), [bkT], [MKT])
        MQL = [TL(A.bf16(2 * 512)) for _ in range(2)]
        PTL = [TL(A.bf16(512)) for _ in range(4)]
        ftmp = attn_tmp()
        for qb in range(DBG_BLOCKS or T // 512):
            c0 = qb * 512
            MQ = MQL[qb % 2]
            mq3 = MQ.ap.rearrange("p (c t) -> p c t", c=2)
            P.dma(mq3, FMT_D[7:9, :, c0:c0 + 512].rearrange("c p t -> p c t"), reads=[b_FMT], writes=[MQ])
            steps = [(h, mt) for h in range(4) for mt in range(2)]

            def st_a(i, mq3=mq3, MQ=MQ):
                h, mt = steps[i]
                ch, pb = h // 2, 64 * (h % 2)
                bk = nb()
                mm(bk.ap, mkt3[pb:pb + 64, ch, mt * 128:(mt + 1) * 128], mq3[pb:pb + 64, ch, :], True, True, [MKT, MQ], [bk])
                PT = PTL[i % 4]
                act(PT.ap, bk.ap, AF.Exp, [bk], [PT], scale=0.125)

            def st_b(i):
                h, mt = steps[i]
                PT = PTL[i % 4]
                for qt in range(4):
                    mm(accs[qt].ap[:, h * 65:(h + 1) * 65], PT.ap[:, qt * 128:(qt + 1) * 128], mv4[:, mt, h, 0:65], mt == 0, mt == 1, [PT, MV], [accs[qt]])

            LK = 2
            for i in range(len(steps) + LK):
                if i < len(steps):
                    st_a(i)
                if i >= LK:
                    st_b(i - LK)
            attn_finish(accs, 768, c0, ftmp)
        rot["set"] = list(range(8))

    def phase_dsa(l):
        P.barrier()
        A.off = BASE
        wl_state.clear()
        rot["set"] = [4, 5, 6, 7]
        accs = banks[0:4]
        IKT = TL(A.bf16(T))
        KT = TL(A.bf16(2 * T))
        kt3 = KT.ap.rearrange("p (c t) -> p c t", c=2)
        VP = TL(A.bf16(NT * 264))
        vp4 = VP.ap.rearrange("p (t h d) -> p t h d", t=NT, h=4)
        WI = TL(A.f32(NT * 4))
        wi3 = WI.ap.rearrange("p (t h) -> p t h", t=NT)
        P.dma(IKT.ap, FMT_D[2], reads=[b_FMT], writes=[IKT])
        P.dma(kt3, FMT_D[5:7].rearrange("c p t -> p c t"), reads=[b_FMT], writes=[KT])
        P.dma(VP.ap.rearrange("p (t n) -> p t n", t=NT), VP_D, reads=[b_VP], writes=[VP])
        P.dma(wi3, WI_D, reads=[b_WI], writes=[WI])
        ISC = TL(A.f32(4 * T))
        isc3 = ISC.ap.rearrange("p (q s) -> p q s", q=4)
        MB = TL(A.bf16(4 * T))
        mb3 = MB.ap.rearrange("p (q s) -> p q s", q=4)
        JNK = TL(A.fp8(T))
        JNK2 = TL(A.fp8(T))
        IQL = [TL(A.bf16(2 * 512)) for _ in range(2)]
        QTL = [TL(A.bf16(2 * 512)) for _ in range(2)]
        RL = [TL(A.f32(512)) for _ in range(4)]
        PTL = [TL(A.bf16(512)) for _ in range(3)]
        MX = TL(A.f32(4))
        STEP = TL(A.f32(NITER * 4))
        st3 = STEP.ap.rearrange("p (i q) -> p i q", i=NITER)
        LOa, LOb = TL(A.f32(2)), TL(A.f32(2))
        MIDa, CNTa, TMPa = TL(A.f32(2)), TL(A.f32(2)), TL(A.f32(2))
        NMIDb, SGNb, TMPb = TL(A.f32(2)), TL(A.f32(2)), TL(A.f32(2))
        ftmp = attn_tmp()
        cnt = {"r": 0, "p": 0}
        nblk = DBG_BLOCKS or T // 512

        def emit_index(qb):
            c0 = qb * 512
            nk = 512 * (qb + 1)
            IQ, QT = IQL[qb % 2], QTL[qb % 2]
            iq3 = IQ.ap.rearrange("p (c t) -> p c t", c=2)
            qt3 = QT.ap.rearrange("p (c t) -> p c t", c=2)
            P.dma(iq3, FMT_D[0:2, :, c0:c0 + 512].rearrange("c p t -> p c t"), reads=[b_FMT], writes=[IQ])
            P.dma(qt3, FMT_D[3:5, :, c0:c0 + 512].rearrange("c p t -> p c t"), reads=[b_FMT], writes=[QT])
            for qt in range(4):
                for kb in range(qb + 1):
                    eng = ("dve", "pool")[kb % 2]
                    dst = isc3[:, qt, kb * 512:(kb + 1) * 512]
                    for h in range(4):
                        ch, pb = h // 2, 64 * (h % 2)
                        bk = nb()
                        mm(bk.ap, iq3[pb:pb + 64, ch, qt * 128:(qt + 1) * 128], IKT.ap[pb:pb + 64, kb * 512:(kb + 1) * 512], True, True, [IQ, IKT], [bk])
                        wcol = wi3[:, qb * 4 + qt, h:h + 1]
                        if h == 0:
                            ts("dve", dst, bk.ap, 0.0, wcol, ALU.max, ALU.mult, [bk, WI], [ISC])
                            continue
                        R = RL[cnt["r"] % 4]
                        cnt["r"] += 1
                        act(R.ap, bk.ap, AF.Relu, [bk], [R])
                        if eng == "dve":
                            stt("dve", dst, R.ap, wcol, dst, ALU.mult, ALU.add, [R, WI, ISC], [ISC])
                        else:
                            ts("pool", R.ap, R.ap, wcol, 1.0, ALU.mult, ALU.mult, [R, WI], [R])
                            tt("pool", dst, dst, R.ap, ALU.add, [R, ISC], [ISC])
            for qt in range(4):
                red("dve", MX.ap[:, qt:qt + 1], isc3[:, qt, 0:nk], ALU.max, [ISC], [MX], apply_absolute_value=True)
            for qt in range(4):
                tt(("dve", "pool")[qt % 2], isc3[:, qt, c0:c0 + 512], isc3[:, qt, c0:c0 + 512], NEGM[:, qt, :], ALU.add, [ISC, CC], [ISC])

        def emit_bisect(qb):
            nk = 512 * (qb + 1)
            ts("dve", MX.ap, MX.ap, 1.0001, 1e-20, ALU.mult, ALU.add, [MX], [MX])
            tt("dve", st3, bc(MX.ap, 1, [128, NITER, 4]), bc(POW2[:, 0:NITER], 2, [128, NITER, 4]), ALU.mult, [MX, CC], [STEP])
            ts("dve", LOa.ap, MX.ap[:, 0:2], -1.0, None, ALU.mult, None, [MX], [LOa])
            ts("dve", LOb.ap, MX.ap[:, 2:4], -1.0, None, ALU.mult, None, [MX], [LOb])
            for it in range(NITER):
                tt("dve", MIDa.ap, LOa.ap, st3[:, it, 0:2], ALU.add, [LOa, STEP], [MIDa])
                stt("dve", NMIDb.ap, LOb.ap, -1.0, st3[:, it, 2:4], ALU.mult, ALU.subtract, [LOb, STEP], [NMIDb])
                for q2 in range(2):
                    ts("dve", JNK.ap[:, 0:nk], isc3[:, q2, 0:nk], MIDa.ap[:, q2:q2 + 1], None, ALU.is_ge, ALU.add, [ISC, MIDa], [JNK, CNTa],
                       accum_out=CNTa.ap[:, q2:q2 + 1])
                    act(JNK2.ap[:, 0:nk], isc3[:, 2 + q2, 0:nk], AF.Sign, [ISC, NMIDb], [JNK2, SGNb], bias=NMIDb.ap[:, q2:q2 + 1],
                        accum_out=SGNb.ap[:, q2:q2 + 1])
                stt("dve", TMPa.ap, CNTa.ap, 255.5, st3[:, it, 0:2], ALU.is_ge, ALU.mult, [CNTa, STEP], [TMPa])
                tt("dve", LOa.ap, LOa.ap, TMPa.ap, ALU.add, [LOa, TMPa], [LOa])
                stt("dve", TMPb.ap, SGNb.ap, float(511 - nk), st3[:, it, 2:4], ALU.is_ge, ALU.mult, [SGNb, STEP], [TMPb])
                tt("dve", LOb.ap, LOb.ap, TMPb.ap, ALU.add, [LOb, TMPb], [LOb])
            LOs = [(LOa, 0), (LOa, 1), (LOb, 0), (LOb, 1)]
            for qt in range(4):
                ts("dve", mb3[:, qt, 0:nk], isc3[:, qt, 0:nk], LOs[qt][0].ap[:, LOs[qt][1]:LOs[qt][1] + 1], -30000.0, ALU.is_lt, ALU.mult,
                   [ISC, LOs[qt][0]], [MB])

        def emit_attn(qb):
            c0 = qb * 512
            QT = QTL[qb % 2]
            qt3 = QT.ap.rearrange("p (c t) -> p c t", c=2)
            nkt = 4 * (qb + 1)
            steps = [(h, k_) for h in range(4) for k_ in range(nkt)]
            slot = {}

            def st_a(i):
                h, kt_ = steps[i]
                ch, pb = h // 2, 64 * (h % 2)
                bk = nb()
                mm(bk.ap, kt3[pb:pb + 64, ch, kt_ * 128:(kt_ + 1) * 128], qt3[pb:pb + 64, ch, :], True, False, [KT, QT], [bk])
                for qt in range(4):
                    mm(bk.ap[:, qt * 128:(qt + 1) * 128], mb3[:, qt, kt_ * 128:(kt_ + 1) * 128], IDB, False, qt == 3, [MB, CB16], [bk])
                PT = PTL[cnt["p"] % 3]
                cnt["p"] += 1
                slot[i] = PT
                act(PT.ap, bk.ap, AF.Exp, [bk], [PT], scale=0.125)

            def st_b(i):
                h, kt_ = steps[i]
                PT = slot.pop(i)
                for qt in range(4):
                    mm(accs[qt].ap[:, h * 65:(h + 1) * 65], PT.ap[:, qt * 128:(qt + 1) * 128], vp4[:, kt_, h, 0:65], kt_ == 0, kt_ == nkt - 1,
                       [PT, VP], [accs[qt]])

            LK = 2
            for i in range(len(steps) + LK):
                if i < len(steps):
                    st_a(i)
                if i >= LK:
                    st_b(i - LK)
            attn_finish(accs, 512, c0, ftmp)

        emit_index(0)
        emit_bisect(0)
        for qb in range(1, nblk):
            emit_index(qb)
            emit_attn(qb - 1)
            emit_bisect(qb)
        emit_attn(nblk - 1)
        rot["set"] = list(range(8))

    def phase_out(l, x_src, b_xsrc):
        PP = PPs[l]
        P.barrier()
        A.off = BASE
        wl_state.clear()
        WO = TL(A.bf16(KC * D))
        wo3 = WO.ap.rearrange("p (c n) -> p c n", c=KC)
        a0 = A.off
        load_weight(WO, wo3, wout_d[l], D, D, lambda c: (PP.ap[:, PP_SSDNG + c:PP_SSDNG + c + 1], PP) if c < 4 else None)
        P.barrier()
        A.off = a0
        A.mark()
        for blk in range(DBG_BLOCKS or T // 512):
            A.reset()
            c0 = blk * 512
            XB = TL(A.f32(KC * 512))
            xb3 = XB.ap.rearrange("p (c t) -> p c t", c=KC)
            YB = TL(A.bf16(KC * 512))
            yb3 = YB.ap.rearrange("p (c t) -> p c t", c=KC)
            P.dma(xb3, x_src[:, c0:c0 + 512].rearrange("(c p) t -> p c t", p=128), reads=[b_xsrc], writes=[XB])
            P.dma(yb3, YM_D[:, c0:c0 + 512].rearrange("(c p) t -> p c t", p=128), reads=[b_YM], writes=[YB])
            XO = TL(A.f32(KC * 512))
            xo3 = XO.ap.rearrange("p (c t) -> p c t", c=KC)
            for n in range(KC):
                bk = nb()
                for k in range(KC):
                    mm(bk.ap, wo3[:, k, n * 128:(n + 1) * 128], yb3[:, k, :], k == 0, k == KC - 1, [WO, YB], [bk])
                tt("dve", xo3[:, n, :], bk.ap, xb3[:, n, :], ALU.add, [bk, XB], [XO])
            P.dma(X1_D[:, c0:c0 + 512].rearrange("(c p) t -> p c t", p=128), xo3, reads=[XO], writes=[b_X1])

    def phase_ffn(l, dst_d, b_dst, is_out):
        PP = PPs[l]
        FB = 256
        NF = 2 * DFF // 128
        P.barrier()
        A.off = BASE
        wl_state.clear()
        rot["set"] = list(range(8))
        WUP = TL(A.bf16(KC * 2 * DFF))
        wu3 = WUP.ap.rearrange("p (c n) -> p c n", c=KC)
        WDN = TL(A.bf16(22 * D))
        wd3 = WDN.ap.rearrange("p (c n) -> p c n", c=22)
        a0 = A.off
        load_weight(WUP, wu3, wup_d[l], D, 2 * DFF, lambda c: (PP.ap[:, PP_FFNG + c:PP_FFNG + c + 1], PP))
        load_weight(WDN, wd3, wdn_d[l], DFF, D, None)
        P.barrier()
        A.off = a0
        URH = TL(A.bf16(NF * 2))
        urh3 = URH.ap.rearrange("p (c t) -> p c t", c=NF)
        ms("pool", URH.ap, 0.0, [URH])
        NU = 5
        TCL = [TL(A.f32(FB)) for _ in range(3)]
        URL = [TL(A.bf16(FB + 2)) for _ in range(NU)]
        SGL = [TL(A.bf16(FB)) for _ in range(4)]
        Sc = [TL(A.bf16(FB)) for _ in range(22)]
        A.mark()
        parts = [(f, p_) for f in range(22) for p_ in range(2)]
        LOOK = 3
        for blk in range((DBG_BLOCKS * 2) if DBG_BLOCKS else T // FB):
            A.reset()
            c0 = blk * FB
            XB = TL(A.f32(KC * FB))
            xb3 = XB.ap.rearrange("p (c t) -> p c t", c=KC)
            P.dma(xb3, X1_D[:, c0:c0 + FB].rearrange("(c p) t -> p c t", p=128), reads=[b_X1], writes=[XB])
            HT = TL(A.bf16(KC * FB))
            HT.ap = HT.ap.rearrange("p (c t) -> p c t", c=KC)
            rmsnorm_block(TL(xb3, XB.b), HT, FB)

            def emit_up(i):
                f, part = parts[i]
                fc = f + 22 * part
                UR = URL[i % NU]
                bk = nb()
                for k in range(KC):
                    mm(bk.ap[:, 0:FB], wu3[:, k, fc * 128:(fc + 1) * 128], HT.ap[:, k, :], k == 0, k == KC - 1, [WUP, HT], [bk])
                cp("pool", UR.ap[:, 0:2], urh3[:, fc, :], [URH], [UR])
                cp("act", UR.ap[:, 2:FB + 2], bk.ap[:, 0:FB], [bk], [UR])
                cp("pool", urh3[:, fc, :], UR.ap[:, FB:FB + 2], [UR], [URH])

            def emit_conv(i):
                f, part = parts[i]
                fc = f + 22 * part
                UR = URL[i % NU]
                SG = SGL[f % 4]
                TC = TCL[i % 3]
                w_ = [PP.ap[:, PP_FCW + j * NF + fc:PP_FCW + j * NF + fc + 1] for j in range(3)]
                ts("dve", TC.ap, UR.ap[:, 0:FB], w_[0], None, ALU.mult, None, [UR, PP], [TC])
                stt("dve", TC.ap, UR.ap[:, 1:FB + 1], w_[1], TC.ap, ALU.mult, ALU.add, [UR, PP, TC], [TC])
                stt("dve", TC.ap, UR.ap[:, 2:FB + 2], w_[2], TC.ap, ALU.mult, ALU.add, [UR, PP, TC], [TC])
                bias = PP.ap[:, PP_FCB + fc:PP_FCB + fc + 1]
                if part == 0:
                    act(SG.ap, TC.ap, AF.Silu, [TC, PP], [SG], bias=bias)
                else:
                    stt("dve", Sc[f].ap, TC.ap, bias, SG.ap, ALU.add, ALU.mult, [TC, PP, SG], [Sc[f]])

            for i in range(len(parts) + LOOK):
                if i < len(parts):
                    emit_up(i)
                if i >= LOOK:
                    emit_conv(i - LOOK)
            for n in range(KC):
                bk = nb()
                for f in range(22):
                    mm(bk.ap[:, 0:FB], wd3[:, f, n * 128:(n + 1) * 128], Sc[f].ap, f == 0, f == 21, [WDN, Sc[f]], [bk])
                tt("dve", xb3[:, n, :], bk.ap[:, 0:FB], xb3[:, n, :], ALU.add, [bk, XB], [XB])
            P.dma(dst_d[:, c0:c0 + FB].rearrange("(c p) t -> p c t", p=128), xb3, reads=[XB], writes=[b_dst], is_output=is_out)
        rot["set"] = list(range(8))

    x_src, b_xsrc = xT_d, Buf()
    for li, l in enumerate(layers):
        last = li == len(layers) - 1
        phase_A(l, x_src, b_xsrc)
        if upto == "A":
            break
        phase_mem(l)
        if upto == "mem":
            break
        phase_dsa(l)
        if upto == "dsa":
            break
        phase_out(l, x_src, b_xsrc)
        if upto == "out":
            break
        if last:
            phase_ffn(l, yT_d, b_Y, True)
        else:
            phase_ffn(l, X2_D, b_X2, False)
            x_src, b_xsrc = X2_D, b_X2
    if upto != "all":
        DUM = TL(A.f32(8))
        ms("pool", DUM.ap, 0.0, [DUM])
        P.dma(yT_d[0:128, 0:8], DUM.ap, reads=[DUM], is_output=True)
        P.barrier()
    P.emit()
    return nc


def _consts():
    c = np.zeros((128, NCC), np.float32)
    i = np.arange(128)
    c[:, CC_ID:CC_ID + 128] = np.eye(128, dtype=np.float32)
    c[:, CC_U:CC_U + 128] = (i[:, None] <= i[None, :]).astype(np.float32)
    c[:, CC_SL:CC_SL + 128] = (i[:, None] > i[None, :]).astype(np.float32)
    c[:, CC_ONES:CC_ONES + 128] = 1.0
    s = np.arange(512)
    for j in range(4):
        c[:, CC_NEG + j * 512:CC_NEG + (j + 1) * 512] = np.where(s[None, :] > (128 * j + i)[:, None], -1e30, 0.0)
    c[:, CC_IF:CC_IF + 8] = (500000.0 ** (-(np.arange(8, dtype=np.float32) * 2.0 / 16.0)))[None, :]
    c[:, CC_P2:CC_P2 + 32] = (2.0 ** -np.arange(32, dtype=np.float64))[None, :]
    return c


def _prep_inputs(inp):
    w_in = inp["w_in"]
    sl = lambda a, b: w_in[:, :, a:b]
    w_in_r = np.concatenate([sl(512, 1536), sl(0, 512), sl(2312, 2568), sl(2568, 2632), sl(2568, 2632), sl(1544, 1800), sl(1800, 2056),
                             sl(2636, 2892), sl(2056, 2312), sl(1536, 1544), sl(2632, 2636)], axis=-1)
    assert w_in_r.shape[-1] == NIN
    pp = np.zeros((NL, 128, NPP), np.float32)
    fm = lambda v, n: v.reshape(NL, n, 128).transpose(0, 2, 1)
    pp[:, :, PP_MIXG:PP_MIXG + 8] = fm(inp["mix_norm_g"], 8)
    pp[:, :, PP_FFNG:PP_FFNG + 8] = fm(inp["ffn_norm_g"], 8)
    pp[:, :, PP_MEMG:PP_MEMG + 8] = fm(inp["mem_norm_g"], 8)
    pp[:, :, PP_SSDNG:PP_SSDNG + 4] = fm(inp["ssd_norm_g"], 4)
    pp[:, :, PP_CONVW:PP_CONVW + 32] = inp["ssd_conv_w"].reshape(NL, 4, 8, 128).transpose(0, 3, 1, 2).reshape(NL, 128, 32)
    pp[:, :, PP_CONVB:PP_CONVB + 8] = fm(inp["ssd_conv_b"], 8)
    pp[:, :, PP_FCW:PP_FCW + 132] = inp["ffn_conv_w"].reshape(NL, 3, 44, 128).transpose(0, 3, 1, 2).reshape(NL, 128, 132)
    pp[:, :, PP_FCB:PP_FCB + 44] = fm(inp["ffn_conv_b"], 44)
    rowp = np.concatenate([inp["ssd_dt_bias"], inp["ssd_a_log"], inp["ssd_d"], inp["attn_q_norm_g"], inp["attn_k_norm_g"],
                           inp["mem_q_norm_g"], inp["mem_k_norm_g"], inp["ssd_conv_b"][:, 0:768]], axis=-1).astype(np.float32)
    assert rowp.shape == (NL, NRP)
    shared = {
        "w_in_r": np.ascontiguousarray(w_in_r, dtype=np.float32),
        "w_mem_kv": np.ascontiguousarray(inp["w_mem_kv"], dtype=np.float32),
        "w_out": np.ascontiguousarray(inp["w_out"], dtype=np.float32),
        "w_up": np.ascontiguousarray(inp["w_up"], dtype=np.float32),
        "w_down": np.ascontiguousarray(inp["w_down"], dtype=np.float32),
        "pp": pp, "rowp": np.ascontiguousarray(rowp), "consts": _consts(),
    }
    idle = {k: (v if k == "consts" else np.zeros_like(v)) for k, v in shared.items()}
    idle["xT"] = np.zeros((D, T), np.float32)
    idle["memT"] = np.zeros((D, 256), np.float32)
    idle["posr"] = np.zeros((128, NT), np.int32)
    maps = []
    for core in range(8):
        if core >= 4:
            maps.append(dict(idle))
            continue
        b = core
        m = dict(shared)
        m["xT"] = np.ascontiguousarray(inp["x"][b].T, dtype=np.float32)
        m["memT"] = np.ascontiguousarray(inp["mem"][b].T, dtype=np.float32)
        m["posr"] = np.ascontiguousarray(inp["pos"][b].reshape(NT, 128).T, dtype=np.int32)
        maps.append(m)
    return maps


_NC_CACHE = {}


def kernel(**inputs):
    inp = {k: np.asarray(v) for k, v in inputs.items()}
    maps = _prep_inputs(inp)
    if "full" not in _NC_CACHE:
        _NC_CACHE["full"] = build()
    nc = _NC_CACHE["full"]
    res = run_bass_kernel_spmd(nc, maps, core_ids=list(range(8)))
    out = np.stack([np.ascontiguousarray(res.results[b]["yT"].T) for b in range(4)], axis=0)
    return out.astype(np.float32)
```

```python
import numpy as np
import concourse.bass as bass
import concourse.mybir as mybir
from concourse.bass_utils import run_bass_kernel_spmd

F32 = mybir.dt.float32
BF16 = mybir.dt.bfloat16
I32 = mybir.dt.int32
FP8 = mybir.dt.int8
ALU = mybir.AluOpType
AF = mybir.ActivationFunctionType
AX = mybir.AxisListType

SAME_ENGINE_SYNC = True
N_DMA_SEMS = 24
SEM_SWITCH = 6000
NITER = 16
DBG_BLOCKS = None
DBG_BLOCKS_A = None
EPS = 1e-6

NL = 2
D = 1024
T = 4096
NT = T // 128
KC = D // 128
DFF = 2816
NFM = 1024
TM_Z, TM_IQ, TM_IK, TM_IK2, TM_Q, TM_K, TM_MQ, TM_V, TM_DT, TM_IW, NTM = 0, 512, 768, 832, 896, 1152, 1408, 1664, 1920, 1928, 1932
NIN = NFM + NTM
PP_MIXG, PP_FFNG, PP_MEMG, PP_SSDNG, PP_CONVW, PP_CONVB, PP_FCW, PP_FCB, NPP = 0, 8, 16, 24, 28, 60, 68, 200, 244
RP_DTB, RP_ALOG, RP_D, RP_QG, RP_KG, RP_MQG, RP_MKG, RP_CB, NRP = 0, 8, 16, 24, 88, 152, 216, 280, 1048
CC_ID, CC_U, CC_SL, CC_ONES, CC_NEG, CC_IF, CC_P2, NCC = 0, 128, 256, 384, 512, 2560, 2568, 2568 + 32


class Buf:
    __slots__ = ("w", "r", "x")

    def __init__(self, excl=False):
        self.w = None
        self.r = {}
        self.x = excl


class TL:
    __slots__ = ("ap", "b")

    def __init__(self, ap, b=None):
        if isinstance(ap, TL):
            self.ap, self.b = ap.ap, ap.b
            return
        self.ap = ap
        self.b = b if b is not None else Buf()

    def v(self, ap):
        return TL(ap, self.b)


def _bufs(xs):
    return [x.b if isinstance(x, TL) else x for x in xs]


class Prog:
    ENGS = ("pe", "act", "dve", "pool", "sp")

    def __init__(self, nc):
        self.nc = nc
        self.streams = {e: [] for e in self.ENGS}
        self.count = {e: 0 for e in self.ENGS}
        self.epoch = {e: 0 for e in self.ENGS}
        self.waited = {e: {} for e in self.ENGS}
        self.dma_val = [0] * N_DMA_SEMS
        self.dma_rr = 0
        self.out_events = []
        self.nops = 0
        self.cut = None
        self.marks = []

    def stage(self, name):
        self.marks.append((name, self.nops))

    def _collect(self, eng, reads, writes, extra=()):
        need = {}

        def add(ev):
            if ev is None:
                return
            k, v = ev
            if need.get(k, 0) < v:
                need[k] = v

        for b in reads:
            add(b.w)
            if b.x:
                for k, v in b.r.items():
                    if k != eng:
                        add((k, v))
        for b in writes:
            add(b.w)
            for k, v in b.r.items():
                add((k, v))
        for ev in extra:
            add(ev)
        waits = []
        for k, v in need.items():
            if k[0] == eng and (eng == "pe" or not SAME_ENGINE_SYNC):
                continue
            if self.waited[eng].get(k, 0) >= v:
                continue
            self.waited[eng][k] = v
            waits.append((k, v))
        return waits

    def _commit(self, ev, reads, writes):
        k, v = ev
        for b in reads:
            if b.r.get(k, 0) < v:
                b.r[k] = v
        for b in writes:
            b.w = ev
            b.r = {}

    def op(self, eng, fn, reads=(), writes=()):
        self.nops += 1
        if self.cut is not None and self.nops > self.cut:
            return None
        reads = _bufs(reads)
        writes = _bufs(writes)
        waits = self._collect(eng, reads, writes)
        self.count[eng] += 1
        key = (eng, self.epoch[eng])
        ev = (key, self.count[eng])
        self.streams[eng].append((waits, fn, key))
        self._commit(ev, reads, writes)
        return ev

    def dma(self, out_ap, in_ap, reads=(), writes=(), q="sp", is_output=False, **kw):
        self.nops += 1
        if self.cut is not None and self.nops > self.cut:
            return None
        reads = _bufs(reads)
        writes = _bufs(writes)
        s = self.dma_rr
        self.dma_rr = (self.dma_rr + 1) % N_DMA_SEMS
        key = ("dma", s)
        extra = [(key, self.dma_val[s])] if self.dma_val[s] > 0 else []
        waits = self._collect(q, reads, writes, extra)
        self.dma_val[s] += 16
        ev = (key, self.dma_val[s])
        self.streams[q].append((waits, lambda e: e.dma_start(out=out_ap, in_=in_ap, **kw), key))
        self._commit(ev, reads, writes)
        if is_output:
            self.out_events.append(ev)
        return ev

    def barrier(self):
        for e in self.ENGS:
            waits = []
            for k2 in self.ENGS:
                key = (k2, self.epoch[k2])
                if k2 != e and self.count[k2] > self.waited[e].get(key, 0):
                    self.waited[e][key] = self.count[k2]
                    waits.append((key, self.count[k2]))
            for s in range(N_DMA_SEMS):
                k = ("dma", s)
                if self.dma_val[s] > self.waited[e].get(k, 0):
                    self.waited[e][k] = self.dma_val[s]
                    waits.append((k, self.dma_val[s]))
            if waits:
                self.streams[e].append((waits, None, None))
        for e in self.ENGS:
            if self.count[e] > SEM_SWITCH:
                self.epoch[e] += 1
                self.count[e] = 0

    def emit(self):
        nc = self.nc
        from contextlib import ExitStack
        with ExitStack() as es:
            sems = {}
            for e in self.ENGS:
                for ep in range(self.epoch[e] + 1):
                    sems[(e, ep)] = es.enter_context(nc.semaphore("s_%s%d" % (e, ep)))
            for i in range(N_DMA_SEMS):
                sems[("dma", i)] = es.enter_context(nc.semaphore("s_dma%d" % i))
            block = es.enter_context(nc.Block())
            fin = {}
            for k, v in self.out_events:
                fin[k] = max(fin.get(k, 0), v)

            def run(name):
                def f(e):
                    for waits, fn, inc in self.streams[name]:
                        for k, v in waits:
                            e.wait_ge(sems[k], v)
                        if fn is None:
                            continue
                        ins = fn(e)
                        ins.then_inc(sems[inc], 16 if inc[0] == "dma" else 1)
                    if name == "sp":
                        for k, v in fin.items():
                            e.wait_ge(sems[k], v)
                return f

            block.sync(run("sp"))
            block.tensor(run("pe"))
            block.scalar(run("act"))
            block.vector(run("dve"))
            block.gpsimd(run("pool"))


class Arena:
    def __init__(self, ap_f32, nwords):
        self.ap = ap_f32
        self.n = nwords
        self.off = 0
        self.mark_ = 0
        self.live = []

    def mark(self):
        self.mark_ = self.off

    def reset(self):
        self.off = self.mark_

    def _alloc(self, w):
        o = self.off
        self.off += w
        assert self.off <= self.n, ("arena overflow", self.off, self.n)
        s_, e_ = o, o + w
        keep, inherit, same = [], [], None
        for (s2, e2, b2) in self.live:
            if e2 <= s_ or s2 >= e_:
                keep.append((s2, e2, b2))
            elif s2 == s_ and e2 == e_:
                same = b2
            else:
                inherit.append(b2)
        if same is not None and not inherit:
            buf = same
        else:
            buf = Buf()
            for b2 in inherit + ([same] if same is not None else []):
                evs = list(b2.r.items()) + ([b2.w] if b2.w is not None else [])
                for k, v in evs:
                    if buf.r.get(k, 0) < v:
                        buf.r[k] = v
        self.live = keep + [(s_, e_, buf)]
        return o, buf

    def f32(self, n):
        o, buf = self._alloc(n)
        return TL(self.ap[:, o:o + n], buf)

    def bf16(self, n):
        w = (n + 1) // 2
        o, buf = self._alloc(w)
        return TL(self.ap[:, o:o + w].bitcast(BF16)[:, 0:n], buf)

    def fp8(self, n):
        w = (n + 3) // 4
        o, buf = self._alloc(w)
        return TL(self.ap[:, o:o + w].bitcast(FP8)[:, 0:n], buf)

    def i32(self, n):
        t = self.f32(n)
        return TL(t.ap.bitcast(I32), t.b)


DBG_CUT = None
LAST_PROG = [None]


def build(layers=(0, 1), upto="all", debug=()):
    nc = bass.Bass("TRN2", target_bir_lowering=False)
    P = Prog(nc)
    P.cut = DBG_CUT
    LAST_PROG[0] = P

    def din(name, shape, dt=F32):
        return nc.dram_tensor(name, list(shape), dt, kind="ExternalInput").ap()

    def dscr(name, shape, dt):
        kind = "ExternalOutput" if name in debug else "Internal"
        return nc.dram_tensor(name, list(shape), dt, kind=kind).ap()

    xT_d = din("xT", [D, T])
    memT_d = din("memT", [D, 256])
    pos_d = din("posr", [128, NT], I32)
    win_d = din("w_in_r", [NL, D, NIN])
    wkv_d = din("w_mem_kv", [NL, D, 512])
    wout_d = din("w_out", [NL, D, D])
    wup_d = din("w_up", [NL, D, 2 * DFF])
    wdn_d = din("w_down", [NL, DFF, D])
    pp_d = din("pp", [NL, 128, NPP])
    rp_d = din("rowp", [NL, NRP])
    cc_d = din("consts", [128, NCC])
    yT_d = nc.dram_tensor("yT", [D, T], F32, kind="ExternalOutput").ap()

    FMT_D = dscr("FMT", [9, 128, T], BF16)
    VP_D = dscr("VP", [128, NT, 264], BF16)
    WI_D = dscr("WI", [128, NT, 4], F32)
    YM_D = dscr("YMIXT", [D, T], BF16)
    X1_D = dscr("X1T", [D, T], F32)
    X2_D = dscr("X2T", [D, T], F32)
    b_FMT, b_VP, b_WI, b_YM, b_X1, b_X2, b_Y = Buf(), Buf(), Buf(), Buf(), Buf(), Buf(), Buf()

    ARW = 50 * 1024
    arena_t = nc.alloc_sbuf_tensor("arena", [128, ARW], F32).ap()
    A = Arena(arena_t, ARW)
    banks = [TL(nc.alloc_psum_tensor("bank%d" % i, [128, 512], F32).ap(), Buf(excl=True)) for i in range(8)]
    rot = {"i": 0, "set": list(range(8))}

    def nb():
        s = rot["set"]
        b = banks[s[rot["i"] % len(s)]]
        rot["i"] += 1
        return b

    def mm(out, lhsT, rhs, start=True, stop=True, reads=(), writes=()):
        return P.op("pe", lambda e: e.matmul(out, lhsT, rhs, start=start, stop=stop), reads, writes)

    def tr(out, in_, ident, reads=(), writes=()):
        return P.op("pe", lambda e: e.transpose(out, in_, ident), reads, writes)

    def act(out, in_, func, reads=(), writes=(), **kw):
        return P.op("act", lambda e: e.activation(out, in_, func, **kw), reads, writes)

    def tt(eng, out, in0, in1, op, reads=(), writes=()):
        return P.op(eng, lambda e: e.tensor_tensor(out, in0, in1, op), reads, writes)

    def ts(eng, out, in0, s1, s2, op0, op1=None, reads=(), writes=(), accum_out=None):
        if accum_out is not None:
            return P.op(eng, lambda e: e.tensor_scalar(out, in0, s1, s2, op0, op1, accum_out=accum_out), reads, writes)
        if op1 is None:
            return P.op(eng, lambda e: e.tensor_scalar(out, in0, s1, None, op0), reads, writes)
        return P.op(eng, lambda e: e.tensor_scalar(out, in0, s1, s2, op0, op1), reads, writes)

    def stt(eng, out, in0, scalar, in1, op0, op1, reads=(), writes=()):
        return P.op(eng, lambda e: e.scalar_tensor_tensor(out, in0, scalar, in1, op0, op1), reads, writes)

    def cp(eng, out, in_, reads=(), writes=()):
        if eng == "act":
            return P.op("act", lambda e: e.activation(out, in_, AF.Copy), reads, writes)
        return P.op(eng, lambda e: e.tensor_copy(out, in_), reads, writes)

    def ms(eng, ap, val, writes=()):
        return P.op(eng, lambda e: e.memset(ap, val), (), writes)

    def red(eng, out, in_, op, reads=(), writes=(), **kw):
        return P.op(eng, lambda e: e.tensor_reduce(out, in_, AX.X, op, **kw), reads, writes)

    def bc(ap, axis, shape):
        return ap.unsqueeze(axis).to_broadcast(list(shape))

    rr = {"i": 0}

    def evac_eng():
        rr["i"] += 1
        return ("act", "dve")[rr["i"] % 2]

    CC = TL(A.f32(NCC))
    P.dma(CC.ap, cc_d, writes=[CC])
    IDENT32 = CC.ap[:, CC_ID:CC_ID + 128]
    U32 = CC.ap[:, CC_U:CC_U + 128]
    SL32 = CC.ap[:, CC_SL:CC_SL + 128]
    ONES32 = CC.ap[:, CC_ONES:CC_ONES + 128]
    NEGM = CC.ap[:, CC_NEG:CC_NEG + 2048].rearrange("p (a b) -> p a b", a=4)
    INVF = CC.ap[:, CC_IF:CC_IF + 8]
    POW2 = CC.ap[:, CC_P2:CC_P2 + 32]
    CB16 = TL(A.bf16(256))
    IDB = CB16.ap[:, 0:128]
    ONESB = CB16.ap[:, 128:256]
    cp("dve", IDB, IDENT32, [CC], [CB16])
    cp("dve", ONESB, ONES32, [CC], [CB16])
    ROW16 = TL(A.bf16(128 + 768))
    ONESROW = ROW16.ap[0:1, 0:128]
    BIASROW = ROW16.ap[0:1, 128:896]
    EPSC = TL(A.f32(1))
    ms("pool", EPSC.ap, EPS, [EPSC])
    MH = TL(A.f32(16))
    ms("pool", MH.ap, -0.5, [MH])
    PPs, RPs = [], []
    for l in range(NL):
        t = TL(A.f32(NPP))
        P.dma(t.ap, pp_d[l], writes=[t])
        PPs.append(t)
        t = TL(A.f32(NRP))
        P.dma(t.ap, rp_d[l].partition_broadcast(128), writes=[t])
        RPs.append(t)
    COS = TL(A.f32(NT * 8))
    SIN = TL(A.f32(NT * 8))
    A.mark()
    BASE = A.off
    if True:
        PI_ = TL(A.i32(NT))
        PF_ = TL(A.f32(NT))
        P.dma(PI_.ap, pos_d, writes=[PI_])
        cp("dve", PF_.ap, PI_.ap, [PI_], [PF_])
        ANG = TL(A.f32(NT * 8))
        KF = TL(A.f32(NT * 8))
        KI = TL(A.i32(NT * 8))
        ang3 = ANG.ap.rearrange("p (a b) -> p a b", a=NT)
        tt("dve", ang3, bc(PF_.ap, 2, [128, NT, 8]), bc(INVF, 1, [128, NT, 8]), ALU.mult, [PF_, CC], [ANG])
        TWO_PI = float(2 * np.pi)
        for dst, shift in ((SIN, 0.0), (COS, float(np.pi / 2))):
            ts("dve", dst.ap, ANG.ap, shift, None, ALU.add, None, [ANG], [dst])
            ts("dve", KF.ap, dst.ap, 1.0 / TWO_PI, None, ALU.mult, None, [dst], [KF])
            cp("dve", KI.ap, KF.ap, [KF], [KI])
            cp("dve", KF.ap, KI.ap, [KI], [KF])
            stt("dve", dst.ap, KF.ap, -6.28125, dst.ap, ALU.mult, ALU.add, [KF, dst], [dst])
            stt("dve", dst.ap, KF.ap, float(-(TWO_PI - 6.28125)), dst.ap, ALU.mult, ALU.add, [KF, dst], [dst])
            ts("dve", KF.ap, dst.ap, float(np.pi), -TWO_PI, ALU.is_gt, ALU.mult, [dst], [KF])
            tt("dve", dst.ap, dst.ap, KF.ap, ALU.add, [dst, KF], [dst])
            ts("dve", KF.ap, dst.ap, float(-np.pi), TWO_PI, ALU.is_lt, ALU.mult, [dst], [KF])
            tt("dve", dst.ap, dst.ap, KF.ap, ALU.add, [dst, KF], [dst])
            ts("dve", dst.ap, dst.ap, float(np.pi), float(-np.pi), ALU.min, ALU.max, [dst], [dst])
            act(dst.ap, dst.ap, AF.Sin, [dst], [dst])
    P.stage("consts_done")
    COS3 = COS.ap.rearrange("p (a b) -> p a b", a=NT)
    SIN3 = SIN.ap.rearrange("p (a b) -> p a b", a=NT)

    wl_state = {}

    def weight_jobs(dst_tl, dst3, src2d, K, N, scale_fn, stg, i0=0):
        CH = 2048
        engs = ("act", "dve", "pool")
        jobs = []
        i = i0
        for c in range(K // 128):
            for n0 in range(0, N, CH):
                n1 = min(N, n0 + CH)

                def job(c=c, n0=n0, n1=n1, i=i):
                    st = stg[i % 3]
                    P.dma(st.ap[:, 0:n1 - n0], src2d[c * 128:(c + 1) * 128, n0:n1], writes=[st])
                    sc = scale_fn(c) if scale_fn is not None else None
                    eng = engs[i % 3]
                    o = dst3[:, c, n0:n1]
                    s_ap = st.ap[:, 0:n1 - n0]
                    rd = [st] + ([sc[1]] if sc is not None else [])
                    if sc is None:
                        cp(eng, o, s_ap, rd, [dst_tl])
                    elif eng == "act":
                        act(o, s_ap, AF.Copy, rd, [dst_tl], scale=sc[0])
                    elif eng == "dve":
                        ts("dve", o, s_ap, sc[0], None, ALU.mult, None, rd, [dst_tl])
                    else:
                        ts("pool", o, s_ap, sc[0], 1.0, ALU.mult, ALU.mult, rd, [dst_tl])
                jobs.append(job)
                i += 1
        return jobs

    def load_weight(dst_tl, dst3, src2d, K, N, scale_fn):
        if wl_state.get("end") != A.off:
            wl_state["stg"] = [TL(A.f32(2048)) for _ in range(3)]
            wl_state["end"] = A.off
        for j in weight_jobs(dst_tl, dst3, src2d, K, N, scale_fn, wl_state["stg"]):
            j()

    def rmsnorm_block(XB, HT, ncols):
        SQ = TL(A.bf16(KC * ncols))
        sq3 = SQ.ap.rearrange("p (c t) -> p c t", c=KC)
        act(sq3, XB.ap, AF.Square, [XB], [SQ])
        bk = nb()
        for c in range(KC):
            mm(bk.ap[:, 0:ncols], ONESB, sq3[:, c, :], c == 0, c == KC - 1, [CB16, SQ], [bk])
        RS = TL(A.f32(ncols))
        act(RS.ap, bk.ap[:, 0:ncols], AF.Ln, [bk, EPSC], [RS], bias=EPSC.ap, scale=1.0 / D)
        act(RS.ap, RS.ap, AF.Exp, [RS], [RS], scale=-0.5)
        tt("dve", HT.ap, XB.ap, bc(RS.ap, 1, [128, KC, ncols]), ALU.mult, [XB, RS], [HT])

    def phase_A(l, x_src, b_xsrc):
        PP, RP = PPs[l], RPs[l]
        P.barrier()
        A.off = BASE
        wl_state.clear()
        WIN = TL(A.bf16(KC * NIN))
        win3 = WIN.ap.rearrange("p (c n) -> p c n", c=KC)
        a0 = A.off
        load_weight(WIN, win3, win_d[l], D, NIN, lambda c: (PP.ap[:, PP_MIXG + c:PP_MIXG + c + 1], PP))
        P.barrier()
        A.off = a0
        P.stage("A_weights_loaded")
        DIAGW = TL(A.bf16(32 * 128))
        dg3 = DIAGW.ap.rearrange("p (a b) -> p a b", a=32)
        for j in range(4):
            for c in range(8):
                ts("pool", dg3[:, j * 8 + c, :], IDB, PP.ap[:, PP_CONVW + j * 8 + c:PP_CONVW + j * 8 + c + 1], 1.0,
                   ALU.mult, ALU.mult, [CB16, PP], [DIAGW])
        DI = TL(A.bf16(8 * 128))
        di3 = DI.ap.rearrange("p (a b) -> p a b", a=8)
        for h in range(8):
            ts("pool", di3[:, h, :], IDB, RP.ap[:, RP_D + h:RP_D + h + 1], 1.0, ALU.mult, ALU.mult, [CB16, RP], [DI])
        ANEG = TL(A.f32(8))
        act(ANEG.ap, RP.ap[:, RP_ALOG:RP_ALOG + 8], AF.Exp, [RP], [ANEG])
        ts("dve", ANEG.ap, ANEG.ap, -1.0, None, ALU.mult, None, [ANEG], [ANEG])
        GN = TL(A.f32(12 * 64))
        gn3 = GN.ap.rearrange("p (a b) -> p a b", a=12)
        for i, off in enumerate((RP_QG, RP_KG, RP_MQG)):
            cp("dve", gn3[:, i * 4:(i + 1) * 4, :], bc(RP.ap[:, off:off + 64], 1, [128, 4, 64]), [RP], [GN])
        ms("pool", ONESROW, 1.0, [ROW16])
        cp("dve", BIASROW, RP.ap[0:1, RP_CB:RP_CB + 768], [RP], [ROW16])
        DTB = RP.ap[:, RP_DTB:RP_DTB + 8]
        H = TL(A.f32(512))
        HB = TL(A.bf16(512))
        ms("pool", H.ap, 0.0, [H])
        ms("pool", HB.ap, 0.0, [HB])
        RAW = TL(A.bf16(8 * 516))
        raw3 = RAW.ap.rearrange("p (c t) -> p c t", c=8)
        ms("pool", RAW.ap, 0.0, [RAW])
        VPB = [TL(A.bf16(4 * 264)) for _ in range(2)]
        for t_ in VPB:
            ms("pool", t_.ap, 1.0, [t_])
        A.mark()
        XSL = [TL(A.bf16(512)) for _ in range(2)]
        BTML = [TL(A.bf16(256)) for _ in range(2)]
        ZSL = [TL(A.bf16(512)) for _ in range(2)]
        DTL = [TL(A.f32(8)) for _ in range(2)]
        ADTL = [TL(A.f32(8)) for _ in range(2)]
        h3v = H.ap.rearrange("p (h d) -> p h d", h=8)
        P.stage("A_layer_consts")

        for blk in range(DBG_BLOCKS_A or DBG_BLOCKS or T // 512):
            A.reset()
            c0 = blk * 512
            XB = TL(A.f32(KC * 512))
            xb3 = XB.ap.rearrange("p (c t) -> p c t", c=KC)
            P.dma(xb3, x_src[:, c0:c0 + 512].rearrange("(c p) t -> p c t", p=128), reads=[b_xsrc], writes=[XB])
            HT = TL(A.bf16(KC * 512))
            HT.ap = HT.ap.rearrange("p (c t) -> p c t", c=KC)
            XB3 = TL(xb3, XB.b)
            rmsnorm_block(XB3, HT, 512)
            ht3 = HT.ap
            P.stage("A_blk%d_norm" % blk)
            for c8 in range(8):
                bk = nb()
                for k in range(KC):
                    mm(bk.ap, win3[:, k, c8 * 128:(c8 + 1) * 128], ht3[:, k, :], k == 0, k == KC - 1, [WIN, HT], [bk])
                cp(evac_eng(), raw3[:, c8, 3:515], bk.ap, [bk], [RAW])
            P.stage("A_blk%d_fmproj" % blk)
            BCT = TL(A.bf16(4 * 512))
            bct3 = BCT.ap.rearrange("p (c t) -> p c t", c=4)
            for j4 in range(4):
                bk = nb()
                for tap in range(4):
                    mm(bk.ap, dg3[:, tap * 8 + 4 + j4, :], raw3[:, 4 + j4, tap:tap + 512], tap == 0, tap == 3, [DIAGW, RAW], [bk])
                act(bct3[:, j4, :], bk.ap, AF.Silu, [bk, PP], [BCT], bias=PP.ap[:, PP_CONVB + 4 + j4:PP_CONVB + 5 + j4])
            P.stage("A_blk%d_bcconv" % blk)
            FMS = TL(A.bf16(9 * 512))
            fms3 = FMS.ap.rearrange("p (c t) -> p c t", c=9)
            YST = TL(A.bf16(4 * 512))
            yst3 = YST.ap.rearrange("p (c t) -> p c t", c=4)
            WIB = TL(A.f32(16))
            wib3 = WIB.ap.rearrange("p (a b) -> p a b", a=4)
            vpb = VPB[blk % 2]
            vpb4 = vpb.ap.rearrange("p (t h d) -> p t h d", t=4, h=4)
            a_tile = A.off
            tstate = {}

            def front(ti):
                    A.off = a_tile
                    o = ti * 128
                    g = blk * 4 + ti
                    bk = nb()
                    for c4 in range(4):
                        for tap in range(4):
                            mm(bk.ap[:, c4 * 128:(c4 + 1) * 128], raw3[:, c4, o + tap:o + tap + 128], dg3[:, tap * 8 + c4, :],
                               tap == 0, False, [RAW, DIAGW], [bk])
                        mm(bk.ap[:, c4 * 128:(c4 + 1) * 128], ONESROW, BIASROW[:, c4 * 128:(c4 + 1) * 128], False, True, [ROW16], [bk])
                    XS = XSL[g % 2]
                    act(XS.ap, bk.ap, AF.Silu, [bk], [XS])
                    bk = nb()
                    for c2 in range(2):
                        for tap in range(4):
                            mm(bk.ap[:, c2 * 128:(c2 + 1) * 128], raw3[:, 4 + c2, o + tap:o + tap + 128], dg3[:, tap * 8 + 4 + c2, :],
                               tap == 0, False, [RAW, DIAGW], [bk])
                        mm(bk.ap[:, c2 * 128:(c2 + 1) * 128], ONESROW, BIASROW[:, 512 + c2 * 128:512 + (c2 + 1) * 128], False, True, [ROW16], [bk])
                    BTM = BTML[g % 2]
                    act(BTM.ap, bk.ap[:, 0:256], AF.Silu, [bk], [BTM])
                    P.stage("A_t%d_xsconv" % g)
                    TMX = TL(A.f32(18 * 64))
                    tmx3 = TMX.ap.rearrange("p (h d) -> p h d", h=18)
                    ZS = ZSL[g % 2]
                    DT = DTL[g % 2]
                    groups = ((TM_Z, 512), (TM_IQ, 384), (TM_Q, 512), (TM_MQ, 512), (TM_DT, 12))
                    for gi, (col0, ncol) in enumerate(groups):
                        bk = nb()
                        for k in range(KC):
                            mm(bk.ap[:, 0:ncol], ht3[:, k, o:o + 128], win3[:, k, NFM + col0:NFM + col0 + ncol], k == 0, k == KC - 1, [HT, WIN], [bk])
                        if gi == 0:
                            act(ZS.ap, bk.ap, AF.Silu, [bk], [ZS])
                        elif gi == 1:
                            cp("act", TMX.ap[:, 0:384], bk.ap[:, 0:384], [bk], [TMX])
                        elif gi == 2:
                            cp("dve", TMX.ap[:, 384:896], bk.ap, [bk], [TMX])
                        elif gi == 3:
                            cp("act", TMX.ap[:, 896:1152], bk.ap[:, 0:256], [bk], [TMX])
                            cp("act", vpb4[:, ti, :, 0:64], bk.ap[:, 256:512].rearrange("p (h d) -> p h d", h=4), [bk], [vpb])
                        else:
                            tt("dve", DT.ap, bk.ap[:, 0:8], DTB, ALU.add, [bk, RP], [DT])
                            ts("dve", wib3[:, ti, :], bk.ap[:, 8:12], 1.0 / 16.0, None, ALU.mult, None, [bk], [WIB])
                    P.stage("A_t%d_tmproj" % g)
                    act(DT.ap, DT.ap, AF.Exp, [DT], [DT])
                    act(DT.ap, DT.ap, AF.Ln, [DT], [DT], bias=1.0)
                    ADT = ADTL[g % 2]
                    tt("dve", ADT.ap, DT.ap, ANEG.ap, ALU.mult, [DT, ANEG], [ADT])
                    P.stage("A_t%d_softplus" % g)
                    SQN = TL(A.f32(12 * 64))
                    sqn3 = SQN.ap.rearrange("p (h d) -> p h d", h=12)
                    SS = TL(A.f32(12))
                    tt("pool", sqn3, tmx3[:, 6:18, :], tmx3[:, 6:18, :], ALU.mult, [TMX], [SQN])
                    red("dve", SS.ap, sqn3, ALU.add, [SQN], [SS])
                    ts("dve", SS.ap, SS.ap, 1.0 / 64.0, EPS, ALU.mult, ALU.add, [SS], [SS])
                    tt("pool", SS.ap, SS.ap, MH.ap[:, 0:12], ALU.pow, [SS, MH], [SS])
                    tt("dve", tmx3[:, 6:18, :], tmx3[:, 6:18, :], bc(SS.ap, 2, [128, 12, 64]), ALU.mult, [TMX, SS], [TMX])
                    tt("dve", tmx3[:, 6:18, :], tmx3[:, 6:18, :], gn3, ALU.mult, [TMX, GN], [TMX])
                    P.stage("A_t%d_norm" % g)
                    TMB = TL(A.bf16(18 * 64))
                    tmb3 = TMB.ap.rearrange("p (h d) -> p h d", h=18)
                    cp("act", TMB.ap, TMX.ap, [TMX], [TMB])
                    x1 = tmx3[:, 0:14, 0:8]
                    x2 = tmx3[:, 0:14, 8:16]
                    cosb = bc(COS3[:, g, :], 1, [128, 14, 8])
                    sinb = bc(SIN3[:, g, :], 1, [128, 14, 8])
                    RT = [TL(A.f32(112)) for _ in range(4)]
                    rt3 = [t_.ap.rearrange("p (h d) -> p h d", h=14) for t_ in RT]
                    tt("dve", rt3[0], x1, cosb, ALU.mult, [TMX, COS], [RT[0]])
                    tt("pool", rt3[1], x2, sinb, ALU.mult, [TMX, SIN], [RT[1]])
                    tt("dve", rt3[2], x2, cosb, ALU.mult, [TMX, COS], [RT[2]])
                    tt("pool", rt3[3], x1, sinb, ALU.mult, [TMX, SIN], [RT[3]])
                    tt("dve", tmb3[:, 0:14, 0:8], rt3[0], rt3[1], ALU.subtract, [RT[0], RT[1]], [TMB])
                    tt("dve", tmb3[:, 0:14, 8:16], rt3[2], rt3[3], ALU.add, [RT[2], RT[3]], [TMB])
                    P.stage("A_t%d_rope" % g)
                    bkA, bkB = nb(), nb()
                    tA = bkA.ap.bitcast(BF16)
                    tB = bkB.ap.bitcast(BF16)
                    for i in range(9):
                        dst = tA[:, i * 128:(i + 1) * 128] if i < 5 else tB[:, (i - 5) * 128:(i - 4) * 128]
                        tr(dst, TMB.ap[:, i * 128:(i + 1) * 128], IDB, [TMB, CB16], [bkA if i < 5 else bkB])
                    cp("act", fms3[:, 0:5, o:o + 128], tA[:, 0:640].rearrange("p (a b) -> p a b", a=5), [bkA], [FMS])
                    cp("dve", fms3[:, 5:9, o:o + 128], tB[:, 0:512].rearrange("p (a b) -> p a b", a=4), [bkB], [FMS])

                    P.stage("A_t%d_transp" % g)
                    tstate[ti] = (XS, BTM, ZS, DT, ADT)
                    if "a_back" not in tstate:
                        tstate["a_back"] = A.off

            def back(ti):
                    A.off = tstate["a_back"]
                    o = ti * 128
                    g = blk * 4 + ti
                    XS, BTM, ZS, DT, ADT = tstate.pop(ti)
                    xs3 = XS.ap.rearrange("p (h d) -> p h d", h=8)
                    bk_cs = nb()
                    mm(bk_cs.ap[:, 0:8], U32, ADT.ap, True, True, [CC, ADT], [bk_cs])
                    mm(bk_cs.ap[:, 8:16], ONES32, ADT.ap, True, True, [CC, ADT], [bk_cs])
                    CST = TL(A.f32(24))
                    cp("act", CST.ap[:, 0:16], bk_cs.ap[:, 0:16], [bk_cs], [CST])
                    tt("dve", CST.ap[:, 16:24], CST.ap[:, 8:16], CST.ap[:, 0:8], ALU.subtract, [CST], [CST])
                    ECS = TL(A.f32(24))
                    act(ECS.ap, CST.ap, AF.Exp, [CST], [ECS])
                    AH = TL(A.f32(8 * 128))
                    ah3 = AH.ap.rearrange("p (h s) -> p h s", h=8)
                    tt("dve", ah3, bc(SL32, 1, [128, 8, 128]), bc(ADT.ap, 2, [128, 8, 128]), ALU.mult, [CC, ADT], [AH])
                    E = TL(A.f32(1024))
                    for g2 in range(2):
                        bk = nb()
                        for r in range(4):
                            mm(bk.ap[:, r * 128:(r + 1) * 128], ah3[:, g2 * 4 + r, :], U32, True, True, [AH, CC], [bk])
                        act(E.ap[:, g2 * 512:(g2 + 1) * 512], bk.ap, AF.Exp, [bk], [E])
                    P.stage("A_t%d_ssdE" % g)
                    bk_cb = nb()
                    for g2 in range(2):
                        mm(bk_cb.ap[:, g2 * 128:(g2 + 1) * 128], bct3[:, g2, o:o + 128], bct3[:, 2 + g2, o:o + 128], True, True, [BCT], [bk_cb])
                    CBM = TL(A.f32(256))
                    cbm3 = CBM.ap.rearrange("p (g l) -> p g l", g=2)
                    tt("dve", cbm3, bk_cb.ap[:, 0:256].rearrange("p (g l) -> p g l", g=2), bc(U32, 1, [128, 2, 128]), ALU.mult, [bk_cb, CC], [CBM])
                    MT = TL(A.bf16(1024))
                    mt3 = MT.ap.rearrange("p (h l) -> p h l", h=8)
                    tt("dve", MT.ap.rearrange("p (g r l) -> p g r l", g=2, r=4), E.ap.rearrange("p (g r l) -> p g r l", g=2, r=4),
                       bc(cbm3, 2, [128, 2, 4, 128]), ALU.mult, [E, CBM], [MT])
                    XDT = TL(A.bf16(512))
                    xdt3 = XDT.ap.rearrange("p (h d) -> p h d", h=8)
                    tt("pool", xdt3, xs3, bc(DT.ap, 2, [128, 8, 64]), ALU.mult, [XS, DT], [XDT])
                    XDEC = TL(A.bf16(512))
                    tt("pool", XDEC.ap.rearrange("p (h d) -> p h d", h=8), xdt3, bc(ECS.ap[:, 16:24], 2, [128, 8, 64]), ALU.mult, [XDT, ECS], [XDEC])
                    P.stage("A_t%d_ssdM" % g)
                    bk_st = nb()
                    for g2 in range(2):
                        mm(bk_st.ap[:, g2 * 256:(g2 + 1) * 256], BTM.ap[:, g2 * 128:(g2 + 1) * 128], XDEC.ap[:, g2 * 256:(g2 + 1) * 256],
                           True, True, [BTM, XDEC], [bk_st])
                    bk_y = nb()
                    for h in range(8):
                        mm(bk_y.ap[:, h * 64:(h + 1) * 64], mt3[:, h, :], xdt3[:, h, :], True, False, [MT, XDT], [bk_y])
                        mm(bk_y.ap[:, h * 64:(h + 1) * 64], di3[:, h, :], xs3[:, h, :], False, True, [DI, XS], [bk_y])
                    bk_yo = nb()
                    for g2 in range(2):
                        mm(bk_yo.ap[:, g2 * 256:(g2 + 1) * 256], bct3[:, 2 + g2, o:o + 128], HB.ap[:, g2 * 256:(g2 + 1) * 256],
                           True, True, [BCT, HB], [bk_yo])
                    Y = TL(A.f32(512))
                    y3 = Y.ap.rearrange("p (h d) -> p h d", h=8)
                    tt("dve", y3, bk_yo.ap.rearrange("p (h d) -> p h d", h=8), bc(ECS.ap[:, 0:8], 2, [128, 8, 64]), ALU.mult, [bk_yo, ECS], [Y])
                    tt("dve", Y.ap, Y.ap, bk_y.ap, ALU.add, [Y, bk_y], [Y])
                    P.stage("A_t%d_ssdY" % g)
                    tt("pool", h3v, h3v, bc(ECS.ap[:, 8:16], 2, [128, 8, 64]), ALU.mult, [H, ECS], [H])
                    tt("dve", H.ap, H.ap, bk_st.ap, ALU.add, [H, bk_st], [H])
                    cp("act", HB.ap, H.ap, [H], [HB])
                    P.stage("A_t%d_ssdH" % g)
                    YG = TL(A.f32(512))
                    tt("dve", YG.ap, Y.ap, ZS.ap, ALU.mult, [Y, ZS], [YG])
                    SSG = TL(A.f32(2))
                    JK = TL(A.bf16(256))
                    for g2 in range(2):
                        act(JK.ap, YG.ap[:, g2 * 256:(g2 + 1) * 256], AF.Square, [YG], [JK, SSG], accum_out=SSG.ap[:, g2:g2 + 1])
                    ts("dve", SSG.ap, SSG.ap, 1.0 / 256.0, EPS, ALU.mult, ALU.add, [SSG], [SSG])
                    tt("pool", SSG.ap, SSG.ap, MH.ap[:, 0:2], ALU.pow, [SSG, MH], [SSG])
                    YN = TL(A.bf16(512))
                    tt("dve", YN.ap.rearrange("p (g d) -> p g d", g=2), YG.ap.rearrange("p (g d) -> p g d", g=2),
                       bc(SSG.ap, 2, [128, 2, 256]), ALU.mult, [YG, SSG], [YN])
                    bkT = nb()
                    tT = bkT.ap.bitcast(BF16)
                    for c4 in range(4):
                        tr(tT[:, c4 * 128:(c4 + 1) * 128], YN.ap[:, c4 * 128:(c4 + 1) * 128], IDB, [YN, CB16], [bkT])
                    cp("act", yst3[:, :, o:o + 128], tT[:, 0:512].rearrange("p (a b) -> p a b", a=4), [bkT], [YST])

            front(0)
            for ti in range(4):
                if ti + 1 < 4:
                    front(ti + 1)
                back(ti)
            cp("pool", raw3[:, :, 0:3], raw3[:, :, 512:515], [RAW], [RAW])
            P.dma(FMT_D[:, :, c0:c0 + 512].rearrange("c p t -> p c t"), fms3, reads=[FMS], writes=[b_FMT])
            P.dma(YM_D[0:512, c0:c0 + 512].rearrange("(c p) t -> p c t", p=128), yst3, reads=[YST], writes=[b_YM])
            P.dma(VP_D[:, blk * 4:(blk + 1) * 4, :], vpb.ap.rearrange("p (t n) -> p t n", t=4), reads=[vpb], writes=[b_VP])
            P.dma(WI_D[:, blk * 4:(blk + 1) * 4, :], wib3, reads=[WIB], writes=[b_WI])

    def attn_tmp():
        return {"YA": [TL(A.bf16(2 * 512)) for _ in range(2)], "O": [TL(A.f32(260)) for _ in range(2)],
                "RD": [TL(A.f32(4)) for _ in range(2)], "YB": [TL(A.bf16(256)) for _ in range(2)], "n": 0}

    def attn_finish(accs, row0, c0, tmp):
        YA = tmp["YA"][tmp["n"] % 2]
        tmp["n"] += 1
        ya3 = YA.ap.rearrange("p (c t) -> p c t", c=2)
        for qt in range(4):
            O, RD, YB = tmp["O"][qt % 2], tmp["RD"][qt % 2], tmp["YB"][qt % 2]
            o3 = O.ap.rearrange("p (h d) -> p h d", h=4)
            cp("act", O.ap, accs[qt].ap[:, 0:260], [accs[qt]], [O])
            P.op("dve", lambda e, RD=RD, o3=o3: e.reciprocal(RD.ap, o3[:, :, 64]), [O], [RD])
            tt("dve", YB.ap.rearrange("p (h d) -> p h d", h=4), o3[:, :, 0:64], bc(RD.ap, 2, [128, 4, 64]), ALU.mult, [O, RD], [YB])
            bkT = nb()
            tT = bkT.ap.bitcast(BF16)
            for c2 in range(2):
                tr(tT[:, c2 * 128:(c2 + 1) * 128], YB.ap[:, c2 * 128:(c2 + 1) * 128], IDB, [YB, CB16], [bkT])
            cp("act", ya3[:, :, qt * 128:(qt + 1) * 128], tT[:, 0:256].rearrange("p (a b) -> p a b", a=2), [bkT], [YA])
        P.dma(YM_D[row0:row0 + 256, c0:c0 + 512].rearrange("(c p) t -> p c t", p=128), ya3, reads=[YA], writes=[b_YM])

    def phase_mem(l):
        PP, RP = PPs[l], RPs[l]
        P.barrier()
        A.off = BASE
        wl_state.clear()
        rot["set"] = [4, 5, 6, 7]
        accs = banks[0:4]
        WKV = TL(A.bf16(KC * 512))
        wkv3 = WKV.ap.rearrange("p (c n) -> p c n", c=KC)
        a0 = A.off
        load_weight(WKV, wkv3, wkv_d[l], D, 512, lambda c: (PP.ap[:, PP_MEMG + c:PP_MEMG + c + 1], PP))
        P.barrier()
        A.off = a0
        MB_ = TL(A.f32(KC * 256))
        mb3 = MB_.ap.rearrange("p (c t) -> p c t", c=KC)
        P.dma(mb3, memT_d.rearrange("(c p) t -> p c t", p=128), writes=[MB_])
        MN = TL(A.bf16(KC * 256))
        MN.ap = MN.ap.rearrange("p (c t) -> p c t", c=KC)
        rmsnorm_block(TL(mb3, MB_.b), MN, 256)
        MKT = TL(A.bf16(2 * 256))
        mkt3 = MKT.ap.rearrange("p (c m) -> p c m", c=2)
        MV = TL(A.bf16(2 * 264))
        ms("pool", MV.ap, 1.0, [MV])
        mv4 = MV.ap.rearrange("p (t h d) -> p t h d", t=2, h=4)
        gk = TL(A.f32(256))
        cp("dve", gk.ap.rearrange("p (h d) -> p h d", h=4), bc(RP.ap[:, RP_MKG:RP_MKG + 64], 1, [128, 4, 64]), [RP], [gk])
        for mt in range(2):
            bk = nb()
            for k in range(KC):
                mm(bk.ap, MN.ap[:, k, mt * 128:(mt + 1) * 128], wkv3[:, k, :], k == 0, k == KC - 1, [MN, WKV], [bk])
            MK = TL(A.f32(256))
            mk3 = MK.ap.rearrange("p (h d) -> p h d", h=4)
            cp("act", MK.ap, bk.ap[:, 0:256], [bk], [MK])
            cp("dve", mv4[:, mt, :, 0:64], bk.ap[:, 256:512].rearrange("p (h d) -> p h d", h=4), [bk], [MV])
            SQ = TL(A.f32(256))
            SS = TL(A.f32(4))
            tt("dve", SQ.ap, MK.ap, MK.ap, ALU.mult, [MK], [SQ])
            red("dve", SS.ap, SQ.ap.rearrange("p (h d) -> p h d", h=4), ALU.add, [SQ], [SS])
            ts("dve", SS.ap, SS.ap, 1.0 / 64.0, EPS, ALU.mult, ALU.add, [SS], [SS])
            tt("pool", SS.ap, SS.ap, MH.ap[:, 0:4], ALU.pow, [SS, MH], [SS])
            tt("dve", mk3, mk3, bc(SS.ap, 2, [128, 4, 64]), ALU.mult, [MK, SS], [MK])
            MKB = TL(A.bf16(256))
            tt("dve", MKB.ap, MK.ap, gk.ap, ALU.mult, [MK, gk], [MKB])
            bkT = nb()
            tT = bkT.ap.bitcast(BF16)
            for c2 in range(2):
                tr(tT[:, c2 * 128:(c2 + 1) * 128], MKB.ap[:, c2 * 128:(c2 + 1) * 128], IDB, [MKB, CB16], [bkT])
            cp("act", mkt3[:, :, mt * 128:(mt + 1) * 128], tT[:, 0:256].rearrange("p (a b) -> p a b", a=2), [bkT], [MKT])
        MQL = [TL(A.bf16(2 * 512)) for _ in range(2)]
        PTL = [TL(A.bf16(512)) for _ in range(4)]
        ftmp = attn_tmp()
        for qb in range(DBG_BLOCKS or T // 512):
            c0 = qb * 512
            MQ = MQL[qb % 2]
            mq3 = MQ.ap.rearrange("p (c t) -> p c t", c=2)
            P.dma(mq3, FMT_D[7:9, :, c0:c0 + 512].rearrange("c p t -> p c t"), reads=[b_FMT], writes=[MQ])
            steps = [(h, mt) for h in range(4) for mt in range(2)]

            def st_a(i, mq3=mq3, MQ=MQ):
                h, mt = steps[i]
                ch, pb = h // 2, 64 * (h % 2)
                bk = nb()
                mm(bk.ap, mkt3[pb:pb + 64, ch, mt * 128:(mt + 1) * 128], mq3[pb:pb + 64, ch, :], True, True, [MKT, MQ], [bk])
                PT = PTL[i % 4]
                act(PT.ap, bk.ap, AF.Exp, [bk], [PT], scale=0.125)

            def st_b(i):
                h, mt = steps[i]
                PT = PTL[i % 4]
                for qt in range(4):
                    mm(accs[qt].ap[:, h * 65:(h + 1) * 65], PT.ap[:, qt * 128:(qt + 1) * 128], mv4[:, mt, h, 0:65], mt == 0, mt == 1, [PT, MV], [accs[qt]])

            LK = 2
            for i in range(len(steps) + LK):
                if i < len(steps):
                    st_a(i)
                if i >= LK:
                    st_b(i - LK)
            attn_finish(accs, 768, c0, ftmp)
        rot["set"] = list(range(8))

    def phase_dsa(l):
        P.barrier()
        A.off = BASE
        wl_state.clear()
        rot["set"] = [4, 5, 6, 7]
        accs = banks[0:4]
        IKT = TL(A.bf16(T))
        KT = TL(A.bf16(2 * T))
        kt3 = KT.ap.rearrange("p (c t) -> p c t", c=2)
        VP = TL(A.bf16(NT * 264))
        vp4 = VP.ap.rearrange("p (t h d) -> p t h d", t=NT, h=4)
        WI = TL(A.f32(NT * 4))
        wi3 = WI.ap.rearrange("p (t h) -> p t h", t=NT)
        P.dma(IKT.ap, FMT_D[2], reads=[b_FMT], writes=[IKT])
        P.dma(kt3, FMT_D[5:7].rearrange("c p t -> p c t"), reads=[b_FMT], writes=[KT])
        P.dma(VP.ap.rearrange("p (t n) -> p t n", t=NT), VP_D, reads=[b_VP], writes=[VP])
        P.dma(wi3, WI_D, reads=[b_WI], writes=[WI])
        ISC = TL(A.f32(4 * T))
        isc3 = ISC.ap.rearrange("p (q s) -> p q s", q=4)
        MB = TL(A.bf16(4 * T))
        mb3 = MB.ap.rearrange("p (q s) -> p q s", q=4)
        JNK = TL(A.fp8(T))
        JNK2 = TL(A.fp8(T))
        IQL = [TL(A.bf16(2 * 512)) for _ in range(2)]
        QTL = [TL(A.bf16(2 * 512)) for _ in range(2)]
        RL = [TL(A.f32(512)) for _ in range(4)]
        PTL = [TL(A.bf16(512)) for _ in range(3)]
        MX = TL(A.f32(4))
        STEP = TL(A.f32(NITER * 4))
        st3 = STEP.ap.rearrange("p (i q) -> p i q", i=NITER)
        LOa, LOb = TL(A.f32(2)), TL(A.f32(2))
        MIDa, CNTa, TMPa = TL(A.f32(2)), TL(A.f32(2)), TL(A.f32(2))
        NMIDb, SGNb, TMPb = TL(A.f32(2)), TL(A.f32(2)), TL(A.f32(2))
        ftmp = attn_tmp()
        cnt = {"r": 0, "p": 0}
        nblk = DBG_BLOCKS or T // 512

        def emit_index(qb):
            c0 = qb * 512
            nk = 512 * (qb + 1)
            IQ, QT = IQL[qb % 2], QTL[qb % 2]
            iq3 = IQ.ap.rearrange("p (c t) -> p c t", c=2)
            qt3 = QT.ap.rearrange("p (c t) -> p c t", c=2)
            P.dma(iq3, FMT_D[0:2, :, c0:c0 + 512].rearrange("c p t -> p c t"), reads=[b_FMT], writes=[IQ])
            P.dma(qt3, FMT_D[3:5, :, c0:c0 + 512].rearrange("c p t -> p c t"), reads=[b_FMT], writes=[QT])
            for qt in range(4):
                for kb in range(qb + 1):
                    eng = ("dve", "pool")[kb % 2]
                    dst = isc3[:, qt, kb * 512:(kb + 1) * 512]
                    for h in range(4):
                        ch, pb = h // 2, 64 * (h % 2)
                        bk = nb()
                        mm(bk.ap, iq3[pb:pb + 64, ch, qt * 128:(qt + 1) * 128], IKT.ap[pb:pb + 64, kb * 512:(kb + 1) * 512], True, True, [IQ, IKT], [bk])
                        wcol = wi3[:, qb * 4 + qt, h:h + 1]
                        if h == 0:
                            ts("dve", dst, bk.ap, 0.0, wcol, ALU.max, ALU.mult, [bk, WI], [ISC])
                            continue
                        R = RL[cnt["r"] % 4]
                        cnt["r"] += 1
                        act(R.ap, bk.ap, AF.Relu, [bk], [R])
                        if eng == "dve":
                            stt("dve", dst, R.ap, wcol, dst, ALU.mult, ALU.add, [R, WI, ISC], [ISC])
                        else:
                            ts("pool", R.ap, R.ap, wcol, 1.0, ALU.mult, ALU.mult, [R, WI], [R])
                            tt("pool", dst, dst, R.ap, ALU.add, [R, ISC], [ISC])
            for qt in range(4):
                red("dve", MX.ap[:, qt:qt + 1], isc3[:, qt, 0:nk], ALU.max, [ISC], [MX], apply_absolute_value=True)
            for qt in range(4):
                tt(("dve", "pool")[qt % 2], isc3[:, qt, c0:c0 + 512], isc3[:, qt, c0:c0 + 512], NEGM[:, qt, :], ALU.add, [ISC, CC], [ISC])

        def emit_bisect(qb):
            nk = 512 * (qb + 1)
            ts("dve", MX.ap, MX.ap, 1.0001, 1e-20, ALU.mult, ALU.add, [MX], [MX])
            tt("dve", st3, bc(MX.ap, 1, [128, NITER, 4]), bc(POW2[:, 0:NITER], 2, [128, NITER, 4]), ALU.mult, [MX, CC], [STEP])
            ts("dve", LOa.ap, MX.ap[:, 0:2], -1.0, None, ALU.mult, None, [MX], [LOa])
            ts("dve", LOb.ap, MX.ap[:, 2:4], -1.0, None, ALU.mult, None, [MX], [LOb])
            for it in range(NITER):
                tt("dve", MIDa.ap, LOa.ap, st3[:, it, 0:2], ALU.add, [LOa, STEP], [MIDa])
                stt("dve", NMIDb.ap, LOb.ap, -1.0, st3[:, it, 2:4], ALU.mult, ALU.subtract, [LOb, STEP], [NMIDb])
                for q2 in range(2):
                    ts("dve", JNK.ap[:, 0:nk], isc3[:, q2, 0:nk], MIDa.ap[:, q2:q2 + 1], None, ALU.is_ge, ALU.add, [ISC, MIDa], [JNK, CNTa],
                       accum_out=CNTa.ap[:, q2:q2 + 1])
                    act(JNK2.ap[:, 0:nk], isc3[:, 2 + q2, 0:nk], AF.Sign, [ISC, NMIDb], [JNK2, SGNb], bias=NMIDb.ap[:, q2:q2 + 1],
                        accum_out=SGNb.ap[:, q2:q2 + 1])
                stt("dve", TMPa.ap, CNTa.ap, 255.5, st3[:, it, 0:2], ALU.is_ge, ALU.mult, [CNTa, STEP], [TMPa])
                tt("dve", LOa.ap, LOa.ap, TMPa.ap, ALU.add, [LOa, TMPa], [LOa])
                stt("dve", TMPb.ap, SGNb.ap, float(511 - nk), st3[:, it, 2:4], ALU.is_ge, ALU.mult, [SGNb, STEP], [TMPb])
                tt("dve", LOb.ap, LOb.ap, TMPb.ap, ALU.add, [LOb, TMPb], [LOb])
            LOs = [(LOa, 0), (LOa, 1), (LOb, 0), (LOb, 1)]
            for qt in range(4):
                ts("dve", mb3[:, qt, 0:nk], isc3[:, qt, 0:nk], LOs[qt][0].ap[:, LOs[qt][1]:LOs[qt][1] + 1], -30000.0, ALU.is_lt, ALU.mult,
                   [ISC, LOs[qt][0]], [MB])

        def emit_attn(qb):
            c0 = qb * 512
            QT = QTL[qb % 2]
            qt3 = QT.ap.rearrange("p (c t) -> p c t", c=2)
            nkt = 4 * (qb + 1)
            steps = [(h, k_) for h in range(4) for k_ in range(nkt)]
            slot = {}

            def st_a(i):
                h, kt_ = steps[i]
                ch, pb = h // 2, 64 * (h % 2)
                bk = nb()
                mm(bk.ap, kt3[pb:pb + 64, ch, kt_ * 128:(kt_ + 1) * 128], qt3[pb:pb + 64, ch, :], True, False, [KT, QT], [bk])
                for qt in range(4):
                    mm(bk.ap[:, qt * 128:(qt + 1) * 128], mb3[:, qt, kt_ * 128:(kt_ + 1) * 128], IDB, False, qt == 3, [MB, CB16], [bk])
                PT = PTL[cnt["p"] % 3]
                cnt["p"] += 1
                slot[i] = PT
                act(PT.ap, bk.ap, AF.Exp, [bk], [PT], scale=0.125)

            def st_b(i):
                h, kt_ = steps[i]
                PT = slot.pop(i)
                for qt in range(4):
                    mm(accs[qt].ap[:, h * 65:(h + 1) * 65], PT.ap[:, qt * 128:(qt + 1) * 128], vp4[:, kt_, h, 0:65], kt_ == 0, kt_ == nkt - 1,
                       [PT, VP], [accs[qt]])

            LK = 2
            for i in range(len(steps) + LK):
                if i < len(steps):
                    st_a(i)
                if i >= LK:
                    st_b(i - LK)
            attn_finish(accs, 512, c0, ftmp)

        emit_index(0)
        emit_bisect(0)
        for qb in range(1, nblk):
            emit_index(qb)
            emit_attn(qb - 1)
            emit_bisect(qb)
        emit_attn(nblk - 1)
        rot["set"] = list(range(8))

    preloaded = {}

    def phase_out(l, x_src, b_xsrc):
        PP = PPs[l]
        P.barrier()
        A.off = BASE
        wl_state.clear()
        WUP = TL(A.bf16(KC * 2 * DFF))
        wu3 = WUP.ap.rearrange("p (c n) -> p c n", c=KC)
        WO = TL(A.bf16(KC * D))
        wo3 = WO.ap.rearrange("p (c n) -> p c n", c=KC)
        stg = [TL(A.f32(2048)) for _ in range(3)]
        for j in weight_jobs(WO, wo3, wout_d[l], D, D, lambda c: (PP.ap[:, PP_SSDNG + c:PP_SSDNG + c + 1], PP) if c < 4 else None, stg):
            j()
        up_jobs = weight_jobs(WUP, wu3, wup_d[l], D, 2 * DFF, lambda c: (PP.ap[:, PP_FFNG + c:PP_FFNG + c + 1], PP), stg, i0=16)
        A.mark()
        nblk_o = DBG_BLOCKS or T // 512
        for blk in range(nblk_o):
            A.reset()
            c0 = blk * 512
            XB = TL(A.f32(KC * 512))
            xb3 = XB.ap.rearrange("p (c t) -> p c t", c=KC)
            YB = TL(A.bf16(KC * 512))
            yb3 = YB.ap.rearrange("p (c t) -> p c t", c=KC)
            P.dma(xb3, x_src[:, c0:c0 + 512].rearrange("(c p) t -> p c t", p=128), reads=[b_xsrc], writes=[XB])
            P.dma(yb3, YM_D[:, c0:c0 + 512].rearrange("(c p) t -> p c t", p=128), reads=[b_YM], writes=[YB])
            XO = TL(A.f32(KC * 512))
            xo3 = XO.ap.rearrange("p (c t) -> p c t", c=KC)
            for n in range(KC):
                bk = nb()
                for k in range(KC):
                    mm(bk.ap, wo3[:, k, n * 128:(n + 1) * 128], yb3[:, k, :], k == 0, k == KC - 1, [WO, YB], [bk])
                tt("dve", xo3[:, n, :], bk.ap, xb3[:, n, :], ALU.add, [bk, XB], [XO])
            P.dma(X1_D[:, c0:c0 + 512].rearrange("(c p) t -> p c t", p=128), xo3, reads=[XO], writes=[b_X1])
            nper = (len(up_jobs) + nblk_o - 1) // nblk_o
            for j in up_jobs[blk * nper:(blk + 1) * nper]:
                j()
        for j in up_jobs[nblk_o * nper:]:
            j()
        preloaded["wup"] = l

    def phase_ffn(l, dst_d, b_dst, is_out):
        PP = PPs[l]
        FB = 256
        NF = 2 * DFF // 128
        P.barrier()
        A.off = BASE
        wl_state.clear()
        rot["set"] = list(range(8))
        WUP = TL(A.bf16(KC * 2 * DFF))
        wu3 = WUP.ap.rearrange("p (c n) -> p c n", c=KC)
        WDN = TL(A.bf16(22 * D))
        wd3 = WDN.ap.rearrange("p (c n) -> p c n", c=22)
        a0 = A.off
        if preloaded.get("wup") != l:
            load_weight(WUP, wu3, wup_d[l], D, 2 * DFF, lambda c: (PP.ap[:, PP_FFNG + c:PP_FFNG + c + 1], PP))
        load_weight(WDN, wd3, wdn_d[l], DFF, D, None)
        P.barrier()
        A.off = a0
        URH = TL(A.bf16(NF * 2))
        urh3 = URH.ap.rearrange("p (c t) -> p c t", c=NF)
        ms("pool", URH.ap, 0.0, [URH])
        NU = 5
        TCL = [TL(A.f32(FB)) for _ in range(3)]
        URL = [TL(A.bf16(FB + 2)) for _ in range(NU)]
        SGL = [TL(A.bf16(FB)) for _ in range(4)]
        Sc = [TL(A.bf16(FB)) for _ in range(22)]
        A.mark()
        parts = [(f, p_) for f in range(22) for p_ in range(2)]
        LOOK = 3
        for blk in range((DBG_BLOCKS * 2) if DBG_BLOCKS else T // FB):
            A.reset()
            c0 = blk * FB
            XB = TL(A.f32(KC * FB))
            xb3 = XB.ap.rearrange("p (c t) -> p c t", c=KC)
            P.dma(xb3, X1_D[:, c0:c0 + FB].rearrange("(c p) t -> p c t", p=128), reads=[b_X1], writes=[XB])
            HT = TL(A.bf16(KC * FB))
            HT.ap = HT.ap.rearrange("p (c t) -> p c t", c=KC)
            rmsnorm_block(TL(xb3, XB.b), HT, FB)

            def emit_up(i):
                f, part = parts[i]
                fc = f + 22 * part
                UR = URL[i % NU]
                bk = nb()
                for k in range(KC):
                    mm(bk.ap[:, 0:FB], wu3[:, k, fc * 128:(fc + 1) * 128], HT.ap[:, k, :], k == 0, k == KC - 1, [WUP, HT], [bk])
                cp("pool", UR.ap[:, 0:2], urh3[:, fc, :], [URH], [UR])
                cp("act", UR.ap[:, 2:FB + 2], bk.ap[:, 0:FB], [bk], [UR])
                cp("pool", urh3[:, fc, :], UR.ap[:, FB:FB + 2], [UR], [URH])

            def emit_conv(i):
                f, part = parts[i]
                fc = f + 22 * part
                UR = URL[i % NU]
                SG = SGL[f % 4]
                TC = TCL[i % 3]
                w_ = [PP.ap[:, PP_FCW + j * NF + fc:PP_FCW + j * NF + fc + 1] for j in range(3)]
                ts("dve", TC.ap, UR.ap[:, 0:FB], w_[0], None, ALU.mult, None, [UR, PP], [TC])
                stt("dve", TC.ap, UR.ap[:, 1:FB + 1], w_[1], TC.ap, ALU.mult, ALU.add, [UR, PP, TC], [TC])
                stt("dve", TC.ap, UR.ap[:, 2:FB + 2], w_[2], TC.ap, ALU.mult, ALU.add, [UR, PP, TC], [TC])
                bias = PP.ap[:, PP_FCB + fc:PP_FCB + fc + 1]
                if part == 0:
                    act(SG.ap, TC.ap, AF.Silu, [TC, PP], [SG], bias=bias)
                else:
                    stt("dve", Sc[f].ap, TC.ap, bias, SG.ap, ALU.add, ALU.mult, [TC, PP, SG], [Sc[f]])

            for i in range(len(parts) + LOOK):
                if i < len(parts):
                    emit_up(i)
                if i >= LOOK:
                    emit_conv(i - LOOK)
            for n in range(KC):
                bk = nb()
                for f in range(22):
                    mm(bk.ap[:, 0:FB], wd3[:, f, n * 128:(n + 1) * 128], Sc[f].ap, f == 0, f == 21, [WDN, Sc[f]], [bk])
                tt("dve", xb3[:, n, :], bk.ap[:, 0:FB], xb3[:, n, :], ALU.add, [bk, XB], [XB])
            P.dma(dst_d[:, c0:c0 + FB].rearrange("(c p) t -> p c t", p=128), xb3, reads=[XB], writes=[b_dst], is_output=is_out)
        rot["set"] = list(range(8))

    x_src, b_xsrc = xT_d, Buf()
    for li, l in enumerate(layers):
        last = li == len(layers) - 1
        phase_A(l, x_src, b_xsrc)
        if upto == "A":
            break
        phase_mem(l)
        if upto == "mem":
            break
        phase_dsa(l)
        if upto == "dsa":
            break
        phase_out(l, x_src, b_xsrc)
        if upto == "out":
            break
        if last:
            phase_ffn(l, yT_d, b_Y, True)
        else:
            phase_ffn(l, X2_D, b_X2, False)
            x_src, b_xsrc = X2_D, b_X2
    if upto != "all":
        DUM = TL(A.f32(8))
        ms("pool", DUM.ap, 0.0, [DUM])
        P.dma(yT_d[0:128, 0:8], DUM.ap, reads=[DUM], is_output=True)
        P.barrier()
    P.emit()
    return nc


def _consts():
    c = np.zeros((128, NCC), np.float32)
    i = np.arange(128)
    c[:, CC_ID:CC_ID + 128] = np.eye(128, dtype=np.float32)
    c[:, CC_U:CC_U + 128] = (i[:, None] <= i[None, :]).astype(np.float32)
    c[:, CC_SL:CC_SL + 128] = (i[:, None] > i[None, :]).astype(np.float32)
    c[:, CC_ONES:CC_ONES + 128] = 1.0
    s = np.arange(512)
    for j in range(4):
        c[:, CC_NEG + j * 512:CC_NEG + (j + 1) * 512] = np.where(s[None, :] > (128 * j + i)[:, None], -1e30, 0.0)
    c[:, CC_IF:CC_IF + 8] = (500000.0 ** (-(np.arange(8, dtype=np.float32) * 2.0 / 16.0)))[None, :]
    c[:, CC_P2:CC_P2 + 32] = (2.0 ** -np.arange(32, dtype=np.float64))[None, :]
    return c


def _prep_inputs(inp):
    w_in = inp["w_in"]
    sl = lambda a, b: w_in[:, :, a:b]
    w_in_r = np.concatenate([sl(512, 1536), sl(0, 512), sl(2312, 2568), sl(2568, 2632), sl(2568, 2632), sl(1544, 1800), sl(1800, 2056),
                             sl(2636, 2892), sl(2056, 2312), sl(1536, 1544), sl(2632, 2636)], axis=-1)
    assert w_in_r.shape[-1] == NIN
    pp = np.zeros((NL, 128, NPP), np.float32)
    fm = lambda v, n: v.reshape(NL, n, 128).transpose(0, 2, 1)
    pp[:, :, PP_MIXG:PP_MIXG + 8] = fm(inp["mix_norm_g"], 8)
    pp[:, :, PP_FFNG:PP_FFNG + 8] = fm(inp["ffn_norm_g"], 8)
    pp[:, :, PP_MEMG:PP_MEMG + 8] = fm(inp["mem_norm_g"], 8)
    pp[:, :, PP_SSDNG:PP_SSDNG + 4] = fm(inp["ssd_norm_g"], 4)
    pp[:, :, PP_CONVW:PP_CONVW + 32] = inp["ssd_conv_w"].reshape(NL, 4, 8, 128).transpose(0, 3, 1, 2).reshape(NL, 128, 32)
    pp[:, :, PP_CONVB:PP_CONVB + 8] = fm(inp["ssd_conv_b"], 8)
    pp[:, :, PP_FCW:PP_FCW + 132] = inp["ffn_conv_w"].reshape(NL, 3, 44, 128).transpose(0, 3, 1, 2).reshape(NL, 128, 132)
    pp[:, :, PP_FCB:PP_FCB + 44] = fm(inp["ffn_conv_b"], 44)
    rowp = np.concatenate([inp["ssd_dt_bias"], inp["ssd_a_log"], inp["ssd_d"], inp["attn_q_norm_g"], inp["attn_k_norm_g"],
                           inp["mem_q_norm_g"], inp["mem_k_norm_g"], inp["ssd_conv_b"][:, 0:768]], axis=-1).astype(np.float32)
    assert rowp.shape == (NL, NRP)
    shared = {
        "w_in_r": np.ascontiguousarray(w_in_r, dtype=np.float32),
        "w_mem_kv": np.ascontiguousarray(inp["w_mem_kv"], dtype=np.float32),
        "w_out": np.ascontiguousarray(inp["w_out"], dtype=np.float32),
        "w_up": np.ascontiguousarray(inp["w_up"], dtype=np.float32),
        "w_down": np.ascontiguousarray(inp["w_down"], dtype=np.float32),
        "pp": pp, "rowp": np.ascontiguousarray(rowp), "consts": _consts(),
    }
    idle = {k: (v if k == "consts" else np.zeros_like(v)) for k, v in shared.items()}
    idle["xT"] = np.zeros((D, T), np.float32)
    idle["memT"] = np.zeros((D, 256), np.float32)
    idle["posr"] = np.zeros((128, NT), np.int32)
    maps = []
    for core in range(8):
        if core >= 4:
            maps.append(dict(idle))
            continue
        b = core
        m = dict(shared)
        m["xT"] = np.ascontiguousarray(inp["x"][b].T, dtype=np.float32)
        m["memT"] = np.ascontiguousarray(inp["mem"][b].T, dtype=np.float32)
        m["posr"] = np.ascontiguousarray(inp["pos"][b].reshape(NT, 128).T, dtype=np.int32)
        maps.append(m)
    return maps


_NC_CACHE = {}


def kernel(**inputs):
    inp = {k: np.asarray(v) for k, v in inputs.items()}
    maps = _prep_inputs(inp)
    if "full" not in _NC_CACHE:
        _NC_CACHE["full"] = build()
    nc = _NC_CACHE["full"]
    res = run_bass_kernel_spmd(nc, maps, core_ids=list(range(8)))
    out = np.stack([np.ascontiguousarray(res.results[b]["yT"].T) for b in range(4)], axis=0)
    return out.astype(np.float32)
```
